# Optimizing a Trainium2 kernel written in Bass

```python
import math
import jax, jax.numpy as jnp
from jax import lax
import numpy as np

D_MODEL = 1024
BATCH = 16
SEQ = 2048
DEPTH = 2

GROUP_WIDTH = D_MODEL // 4
HEAD_DIM = 64
N_HEADS_GROUP = GROUP_WIDTH // HEAD_DIM
D_MIX = 4 * GROUP_WIDTH

MOBA_BLOCK = 256
MOBA_TOPK = 3
MOBA_Q_CHUNK = 32
MLA_Q_RANK = 192
MLA_KV_RANK = 128
MLA_NOPE = 64
MLA_ROPE = 32
MLA_V = HEAD_DIM
MLA_QK = MLA_NOPE + MLA_ROPE
SB_Q_BLOCK = 128
SSM_HEADDIM = 64
SSM_HEADS = GROUP_WIDTH // SSM_HEADDIM
SSM_GROUPS = 2
SSM_STATE = 64
SSM_CONV = 4
SSM_CHUNK = 128
SSM_XBC = GROUP_WIDTH + 2 * SSM_GROUPS * SSM_STATE
D_FF = 2816
FFN_CONV = 3
ROPE_THETA = 10000.0
ATTN_Q_BLOCK = 128
EPS = 1e-6

MOBA_COLS = 3 * GROUP_WIDTH
MLA_COLS = MLA_Q_RANK + MLA_KV_RANK + MLA_ROPE
SB_COLS = 3 * GROUP_WIDTH
SSM_COLS = GROUP_WIDTH + SSM_XBC + SSM_HEADS
IN_COLS = MOBA_COLS + MLA_COLS + SB_COLS + SSM_COLS

kernel_name = "hymba_moba_mla_sb_ssd_convffn"


def _split(x, sizes):
    outs, o = [], 0
    for s in sizes:
        outs.append(x[..., o:o + s])
        o += s
    return outs


def rms_norm(x, g):
    xf = x.astype(jnp.float32)
    y = xf * lax.rsqrt(jnp.mean(xf * xf, axis=-1, keepdims=True) + EPS)
    return (y * g.astype(jnp.float32)).astype(x.dtype)


def rope_tables(positions, dim):
    inv = 1.0 / (ROPE_THETA ** (jnp.arange(0, dim, 2, dtype=jnp.float32) / dim))
    ang = positions.astype(jnp.float32)[..., None] * inv
    return jnp.cos(ang), jnp.sin(ang)


def apply_rope(x, cos, sin):
    x1, x2 = jnp.split(x, 2, axis=-1)
    c = cos[:, :, None, :].astype(x.dtype)
    s = sin[:, :, None, :].astype(x.dtype)
    return jnp.concatenate([x1 * c - x2 * s, x1 * s + x2 * c], axis=-1)


def causal_dwconv(x, w):
    width, ch = w.shape
    return lax.conv_general_dilated(
        x, w[:, None, :], window_strides=(1,), padding=[(width - 1, 0)],
        dimension_numbers=("NWC", "WIO", "NWC"), feature_group_count=ch)


def moba_attention(q, k, v):
    bsz, seq, nh, hd = q.shape
    scale = hd ** -0.5
    nblk = -(-seq // MOBA_BLOCK)
    pad = nblk * MOBA_BLOCK - seq
    kp = jnp.pad(k, ((0, 0), (0, pad), (0, 0), (0, 0)))
    vp = jnp.pad(v, ((0, 0), (0, pad), (0, 0), (0, 0)))
    kbt = kp.reshape(bsz, nblk, MOBA_BLOCK, nh, hd).transpose(0, 3, 1, 2, 4)
    vbt = vp.reshape(bsz, nblk, MOBA_BLOCK, nh, hd).transpose(0, 3, 1, 2, 4)
    kmean = jnp.mean(kbt.astype(jnp.float32), axis=3).astype(k.dtype)
    k_eff = min(MOBA_TOPK, nblk)
    nq = seq // MOBA_Q_CHUNK
    qc = q.reshape(bsz, nq, MOBA_Q_CHUNK, nh, hd).transpose(1, 0, 2, 3, 4)
    blk_ids = jnp.arange(nblk)
    bi = jnp.arange(bsz)[:, None, None, None]
    hi = jnp.arange(nh)[None, :, None, None]

    def one_chunk(args):
        c, qi = args
        cur = (c * MOBA_Q_CHUNK) // MOBA_BLOCK
        gate = jnp.einsum("bqhd,bhnd->bhqn", qi, kmean).astype(jnp.float32)
        gate = jnp.where(blk_ids < cur, gate, -jnp.inf)
        _, idx = lax.top_k(gate, k_eff)
        valid = jnp.arange(k_eff) < cur
        k_sel = kbt[bi, hi, idx]
        v_sel = vbt[bi, hi, idx]
        s_sel = jnp.einsum("bqhd,bhqnjd->bhqnj", qi, k_sel).astype(jnp.float32) * scale
        s_sel = jnp.where(valid[None, None, None, :, None], s_sel, -jnp.inf)
        s_sel = s_sel.reshape(bsz, nh, MOBA_Q_CHUNK, k_eff * MOBA_BLOCK)
        k_own = lax.dynamic_index_in_dim(kbt, cur, axis=2, keepdims=False)
        v_own = lax.dynamic_index_in_dim(vbt, cur, axis=2, keepdims=False)
        s_own = jnp.einsum("bqhd,bhjd->bhqj", qi, k_own).astype(jnp.float32) * scale
        q_local = c * MOBA_Q_CHUNK + jnp.arange(MOBA_Q_CHUNK) - cur * MOBA_BLOCK
        causal = jnp.arange(MOBA_BLOCK)[None, :] <= q_local[:, None]
        s_own = jnp.where(causal, s_own, -jnp.inf)
        p = jax.nn.softmax(jnp.concatenate([s_sel, s_own], axis=-1), axis=-1).astype(v.dtype)
        p_sel = p[..., :k_eff * MOBA_BLOCK].reshape(bsz, nh, MOBA_Q_CHUNK, k_eff, MOBA_BLOCK)
        p_own = p[..., k_eff * MOBA_BLOCK:]
        return (jnp.einsum("bhqnj,bhqnjd->bqhd", p_sel, v_sel)
                + jnp.einsum("bhqj,bhjd->bqhd", p_own, v_own))

    out = lax.map(one_chunk, (jnp.arange(nq), qc))
    return out.transpose(1, 0, 2, 3, 4).reshape(bsz, seq, nh, hd)


def moba_mixer(p, cos, sin, qk_g):
    bsz, seq, _ = p.shape
    q, k, v = [t.reshape(bsz, seq, N_HEADS_GROUP, HEAD_DIM) for t in _split(p, (GROUP_WIDTH,) * 3)]
    q = apply_rope(rms_norm(q, qk_g[0]), cos, sin)
    k = apply_rope(rms_norm(k, qk_g[1]), cos, sin)
    return moba_attention(q, k, v)


def causal_softmax_attention(q, k, v, scale):
    bsz, seq, nh, dk = q.shape
    nb = seq // ATTN_Q_BLOCK
    qb = q.reshape(bsz, nb, ATTN_Q_BLOCK, nh, dk).transpose(1, 0, 2, 3, 4)
    kpos = jnp.arange(seq)

    def one_block(args):
        i, qi = args
        s = jnp.einsum("bqhd,bkhd->bhqk", qi, k).astype(jnp.float32) * scale
        qpos = i * ATTN_Q_BLOCK + jnp.arange(ATTN_Q_BLOCK)
        s = jnp.where(kpos[None, :] <= qpos[:, None], s, -jnp.inf)
        pr = jax.nn.softmax(s, axis=-1).astype(v.dtype)
        return jnp.einsum("bhqk,bkhd->bqhd", pr, v)

    out = lax.map(one_block, (jnp.arange(nb), qb))
    return out.transpose(1, 0, 2, 3, 4).reshape(bsz, seq, nh, v.shape[-1])


def mla_mixer(p, cos, sin, q_norm_g, kv_norm_g, w_uq, w_ukv, qk_g):
    bsz, seq, _ = p.shape
    c_q, c_kv, k_rope = _split(p, (MLA_Q_RANK, MLA_KV_RANK, MLA_ROPE))
    q = jnp.einsum("bsr,rc->bsc", rms_norm(c_q, q_norm_g), w_uq).reshape(bsz, seq, N_HEADS_GROUP, MLA_QK)
    kv = jnp.einsum("bsr,rc->bsc", rms_norm(c_kv, kv_norm_g), w_ukv).reshape(bsz, seq, N_HEADS_GROUP, MLA_NOPE + MLA_V)
    k_nope, v = kv[..., :MLA_NOPE], kv[..., MLA_NOPE:]
    k_pe = jnp.broadcast_to(k_rope[:, :, None, :], (bsz, seq, N_HEADS_GROUP, MLA_ROPE))
    k = jnp.concatenate([k_nope, k_pe], axis=-1)
    q = rms_norm(q, qk_g[0])
    k = rms_norm(k, qk_g[1])
    q = jnp.concatenate([q[..., :MLA_NOPE], apply_rope(q[..., MLA_NOPE:], cos, sin)], axis=-1)
    k = jnp.concatenate([k[..., :MLA_NOPE], apply_rope(k[..., MLA_NOPE:], cos, sin)], axis=-1)
    return causal_softmax_attention(q, k, v, MLA_QK ** -0.5)


def stick_breaking_mixer(p):
    bsz, seq, _ = p.shape
    q, k, v = [t.reshape(bsz, seq, N_HEADS_GROUP, HEAD_DIM) for t in _split(p, (GROUP_WIDTH,) * 3)]
    scale = HEAD_DIM ** -0.5
    nb = seq // SB_Q_BLOCK
    qb = q.reshape(bsz, nb, SB_Q_BLOCK, N_HEADS_GROUP, HEAD_DIM).transpose(1, 0, 2, 3, 4)
    kpos = jnp.arange(seq)

    def one_block(args):
        i, qi = args
        z = jnp.einsum("bqhd,bkhd->bhqk", qi, k).astype(jnp.float32) * scale
        qpos = i * SB_Q_BLOCK + jnp.arange(SB_Q_BLOCK)
        strict = kpos[None, :] < qpos[:, None]
        log_beta = jax.nn.log_sigmoid(z)
        log_1mb = jnp.where(strict, jax.nn.log_sigmoid(-z), 0.0)
        tail = lax.cumsum(log_1mb, axis=3, reverse=True) - log_1mb
        a = jnp.where(strict, jnp.exp(log_beta + tail), 0.0).astype(v.dtype)
        return jnp.einsum("bhqk,bkhd->bqhd", a, v)

    out = lax.map(one_block, (jnp.arange(nb), qb))
    return out.transpose(1, 0, 2, 3, 4).reshape(bsz, seq, N_HEADS_GROUP, HEAD_DIM)


def ssd_chunked(xh, dt, a_head, bm, cm):
    bsz, seq, nh, hp = xh.shape
    ns = bm.shape[-1]
    nc, cl = seq // SSM_CHUNK, SSM_CHUNK
    x = (xh * dt[..., None]).reshape(bsz, nc, cl, nh, hp)
    a = (dt * a_head).reshape(bsz, nc, cl, nh).transpose(0, 3, 1, 2)
    bc = bm.reshape(bsz, nc, cl, nh, ns)
    cc = cm.reshape(bsz, nc, cl, nh, ns)
    a_cum = jnp.cumsum(a, axis=-1)
    tri = jnp.tril(jnp.ones((cl, cl), dtype=bool))
    seg = a_cum[..., :, None] - a_cum[..., None, :]
    lmat = jnp.where(tri, jnp.exp(jnp.where(tri, seg, 0.0)), 0.0)
    y_diag = jnp.einsum("bclhn,bcshn,bhcls,bcshp->bclhp", cc, bc, lmat, x)
    decay_states = jnp.exp(a_cum[..., -1:] - a_cum)
    states = jnp.einsum("bclhn,bhcl,bclhp->bchpn", bc, decay_states, x)
    chunk_decay = jnp.exp(a_cum[..., -1])

    def step(h, inp):
        s_c, d_c = inp
        return d_c[..., None, None] * h + s_c, h

    h0 = jnp.zeros((bsz, nh, hp, ns), jnp.float32)
    _, prev = lax.scan(step, h0, (states.transpose(1, 0, 2, 3, 4), chunk_decay.transpose(2, 0, 1)))
    prev = prev.transpose(1, 0, 2, 3, 4)
    y_off = jnp.einsum("bclhn,bchpn,bhcl->bclhp", cc, prev, jnp.exp(a_cum))
    return (y_diag + y_off).reshape(bsz, seq, nh, hp)


def mamba2_mixer(p, conv_w, conv_b, dt_bias, a_log, d_skip, norm_g):
    bsz, seq, _ = p.shape
    z, xbc, dt_raw = _split(p, (GROUP_WIDTH, SSM_XBC, SSM_HEADS))
    xbc = jax.nn.silu(causal_dwconv(xbc, conv_w) + conv_b)
    xs, bm, cm = _split(xbc, (GROUP_WIDTH, SSM_GROUPS * SSM_STATE, SSM_GROUPS * SSM_STATE))
    rep = SSM_HEADS // SSM_GROUPS
    xh = xs.reshape(bsz, seq, SSM_HEADS, SSM_HEADDIM).astype(jnp.float32)
    bm = jnp.repeat(bm.reshape(bsz, seq, SSM_GROUPS, SSM_STATE), rep, axis=2).astype(jnp.float32)
    cm = jnp.repeat(cm.reshape(bsz, seq, SSM_GROUPS, SSM_STATE), rep, axis=2).astype(jnp.float32)
    dt = jax.nn.softplus(dt_raw.astype(jnp.float32) + dt_bias.astype(jnp.float32))
    a_head = -jnp.exp(a_log.astype(jnp.float32))
    y = ssd_chunked(xh, dt, a_head, bm, cm) + d_skip.astype(jnp.float32)[:, None] * xh
    y = y.reshape(bsz, seq, GROUP_WIDTH).astype(p.dtype)
    return rms_norm(y * jax.nn.silu(z), norm_g)


def setup_inputs(seed: int = 0) -> dict:
    key = jax.random.key(seed)
    ks = jax.random.split(key, 24)
    f32 = jnp.float32
    L = DEPTH

    def normal(k, shape, scale):
        return jax.random.normal(k, shape, f32) * scale

    def gain(k, shape):
        return 1.0 + 0.1 * jax.random.normal(k, shape, f32)

    x = normal(ks[0], (BATCH, SEQ, D_MODEL), 1.0)
    positions = (jax.random.randint(ks[1], (BATCH, 1), 0, 4096, dtype=jnp.int32)
                 + jnp.arange(SEQ, dtype=jnp.int32)[None, :])
    dt0 = jnp.exp(jax.random.uniform(ks[13], (L, SSM_HEADS), f32, math.log(1e-3), math.log(1e-1)))
    return {
        "x": x,
        "positions": positions,
        "mix_norm_g": gain(ks[2], (L, D_MODEL)),
        "w_in": normal(ks[3], (L, D_MODEL, IN_COLS), D_MODEL ** -0.5),
        "moba_qk_g": gain(ks[4], (L, 2, HEAD_DIM)),
        "mla_q_norm_g": gain(ks[5], (L, MLA_Q_RANK)),
        "mla_kv_norm_g": gain(ks[6], (L, MLA_KV_RANK)),
        "mla_w_uq": normal(ks[7], (L, MLA_Q_RANK, N_HEADS_GROUP * MLA_QK), MLA_Q_RANK ** -0.5),
        "mla_w_ukv": normal(ks[8], (L, MLA_KV_RANK, N_HEADS_GROUP * (MLA_NOPE + MLA_V)), MLA_KV_RANK ** -0.5),
        "mla_qk_g": gain(ks[9], (L, 2, MLA_QK)),
        "ssm_conv_w": normal(ks[10], (L, SSM_CONV, SSM_XBC), SSM_CONV ** -0.5),
        "ssm_conv_b": normal(ks[11], (L, SSM_XBC), 0.02),
        "ssm_dt_bias": dt0 + jnp.log(-jnp.expm1(-dt0)),
        "ssm_a_log": jnp.log(jax.random.uniform(ks[12], (L, SSM_HEADS), f32, 1.0, 16.0)),
        "ssm_d": gain(ks[14], (L, SSM_HEADS)),
        "ssm_norm_g": gain(ks[15], (L, GROUP_WIDTH)),
        "head_out_g": gain(ks[16], (L, 3, N_HEADS_GROUP, HEAD_DIM)),
        "w_out": normal(ks[17], (L, D_MIX, D_MODEL), D_MIX ** -0.5),
        "ffn_norm_g": gain(ks[18], (L, D_MODEL)),
        "ffn_w_in": normal(ks[19], (L, D_MODEL, 2 * D_FF), D_MODEL ** -0.5),
        "ffn_conv_w": normal(ks[20], (L, FFN_CONV, 2 * D_FF), FFN_CONV ** -0.5),
        "ffn_conv_b": normal(ks[21], (L, 2 * D_FF), 0.02),
        "ffn_w_out": normal(ks[22], (L, D_FF, D_MODEL), D_FF ** -0.5),
    }


def reference(x, positions, mix_norm_g, w_in, moba_qk_g, mla_q_norm_g, mla_kv_norm_g,
              mla_w_uq, mla_w_ukv, mla_qk_g, ssm_conv_w, ssm_conv_b, ssm_dt_bias, ssm_a_log,
              ssm_d, ssm_norm_g, head_out_g, w_out, ffn_norm_g, ffn_w_in, ffn_conv_w,
              ffn_conv_b, ffn_w_out):
    bsz, seq, _ = x.shape
    cos_a, sin_a = rope_tables(positions, HEAD_DIM)
    cos_m, sin_m = rope_tables(positions, MLA_ROPE)
    for l in range(DEPTH):
        h = rms_norm(x, mix_norm_g[l])
        proj = jnp.einsum("bsd,dc->bsc", h, w_in[l])
        moba_p, mla_p, sb_p, ssm_p = _split(proj, (MOBA_COLS, MLA_COLS, SB_COLS, SSM_COLS))
        o_moba = moba_mixer(moba_p, cos_a, sin_a, moba_qk_g[l])
        o_mla = mla_mixer(mla_p, cos_m, sin_m, mla_q_norm_g[l], mla_kv_norm_g[l],
                          mla_w_uq[l], mla_w_ukv[l], mla_qk_g[l])
        o_sb = stick_breaking_mixer(sb_p)
        o_ssm = mamba2_mixer(ssm_p, ssm_conv_w[l], ssm_conv_b[l], ssm_dt_bias[l],
                             ssm_a_log[l], ssm_d[l], ssm_norm_g[l])
        heads = rms_norm(jnp.stack([o_moba, o_mla, o_sb], axis=2), head_out_g[l])
        mixed = jnp.concatenate([heads.reshape(bsz, seq, 3 * GROUP_WIDTH), o_ssm], axis=-1)
        x = x + jnp.einsum("bsc,cd->bsd", mixed, w_out[l])
        h = rms_norm(x, ffn_norm_g[l])
        u = causal_dwconv(jnp.einsum("bsd,df->bsf", h, ffn_w_in[l]), ffn_conv_w[l]) + ffn_conv_b[l]
        gate, up = _split(u, (D_FF, D_FF))
        x = x + jnp.einsum("bsf,fd->bsd", jax.nn.silu(gate) * up, ffn_w_out[l])
    return x
```

```python
import numpy as np
import ml_dtypes
from contextlib import ExitStack
import concourse.bass as bass
import concourse.mybir as mybir
from concourse.bass_utils import run_bass_kernel_spmd

F32 = mybir.dt.float32
BF16 = mybir.dt.bfloat16
I32 = mybir.dt.int32
AF = mybir.ActivationFunctionType
ALU = mybir.AluOpType
AX = mybir.AxisListType

ENGS = ("pe", "act", "dve", "pool", "sp")
NPOOL = 48
BIG = 32768.0
EPS = 1e-6
THETA = 10000.0


class Tile:
    __slots__ = ("name", "lw", "rd", "const")

    def __init__(self, name, const=False):
        self.name = name
        self.lw = []
        self.rd = {}
        self.const = const


class V:
    __slots__ = ("ap", "t")

    def __init__(self, ap, t):
        self.ap = ap
        self.t = t

    def __getitem__(self, k):
        return V(self.ap[k], self.t)


class Op:
    __slots__ = ("fn", "waits", "signal", "semval", "dma")

    def __init__(self, fn, waits, dma=None):
        self.fn = fn
        self.waits = waits
        self.signal = False
        self.semval = None
        self.dma = dma


class Sched:
    def __init__(self, nc):
        self.nc = nc
        self.ops = {e: [] for e in ENGS}
        self.waited = {e: {} for e in ENGS}
        self.pool_cnt = [0] * NPOOL
        self.pool_next = 0
        self.n = 0

    def tile(self, name=None, const=False):
        self.n += 1
        return Tile(name or f"t{self.n}", const)

    def retile(self, old, n_new):
        mw, mr = {}, {}
        for t in old:
            for d in t.lw:
                k = d[1] if d[0] == "e" else ("d", d[1])
                if k not in mw or mw[k][2] < d[2]:
                    mw[k] = d
            for k, d in t.rd.items():
                if k not in mr or mr[k][2] < d[2]:
                    mr[k] = d
        out = []
        for _ in range(n_new):
            t = self.tile()
            t.lw = list(mw.values())
            t.rd = dict(mr)
            out.append(t)
        return out

    def _need(self, eng, dep, waits):
        if dep is None:
            return
        if dep[0] == "e":
            _, x, idx = dep
            if x == eng and eng == "pe":
                return
            key, val = x, idx
        else:
            _, p, val = dep
            key = ("d", p)
        w = self.waited[eng]
        if w.get(key, -1) >= val:
            return
        w[key] = val
        waits.append(dep)
        if dep[0] == "e":
            self.ops[dep[1]][dep[2]].signal = True

    def _deps(self, eng, reads, writes):
        waits = []
        for v in reads:
            for d in v.t.lw:
                self._need(eng, d, waits)
        for v in writes:
            t = v.t
            for d in t.lw:
                self._need(eng, d, waits)
            for d in list(t.rd.values()):
                self._need(eng, d, waits)
        return waits

    def _mark(self, me, reads, writes):
        rkey = me[1] if me[0] == "e" else ("d", me[1])
        for v in reads:
            t = v.t
            if not t.const:
                t.rd[rkey] = me
        for v in writes:
            t = v.t
            t.lw = [me]
            t.rd = {}

    @staticmethod
    def _vs(xs):
        return [x for x in xs if isinstance(x, V)]

    def op(self, eng, fn, reads=(), writes=()):
        reads = self._vs(reads)
        writes = self._vs(writes)
        waits = self._deps(eng, reads, writes)
        idx = len(self.ops[eng])
        self.ops[eng].append(Op(fn, waits))
        self._mark(("e", eng, idx), reads, writes)

    def dma(self, out, in_, eng="sp", extra_reads=(), **kw):
        p = self.pool_next
        self.pool_next = (self.pool_next + 1) % NPOOL
        reads = [in_] + list(extra_reads)
        waits = self._deps(eng, reads, [out])
        if self.pool_cnt[p] > 0:
            self._need(eng, ("d", p, self.pool_cnt[p]), waits)
        self.pool_cnt[p] += 1
        val = self.pool_cnt[p]
        oap, iap = out.ap, in_.ap

        def fn(e, oap=oap, iap=iap, kw=kw):
            return e.dma_start(out=oap, in_=iap, **kw)
        self.ops[eng].append(Op(fn, waits, dma=(p, val)))
        self._mark(("d", p, val), reads, [out])

    def emit(self):
        nc = self.nc
        with ExitStack() as es:
            esem = {e: es.enter_context(nc.semaphore(f"s_{e}")) for e in ENGS}
            dsem = [es.enter_context(nc.semaphore(f"d_{i}")) for i in range(NPOOL)]
            for e in ENGS:
                c = 0
                for o in self.ops[e]:
                    if o.dma is None and o.signal:
                        c += 1
                        o.semval = c
            block = es.enter_context(nc.Block())
            engobj = {"pe": "tensor", "act": "scalar", "dve": "vector", "pool": "gpsimd", "sp": "sync"}

            def make(ename):
                def body(eng):
                    for o in self.ops[ename]:
                        for d in o.waits:
                            if d[0] == "e":
                                eng.wait_ge(esem[d[1]], self.ops[d[1]][d[2]].semval)
                            else:
                                eng.wait_ge(dsem[d[1]], 16 * d[2])
                        ins = o.fn(eng)
                        if o.dma is not None:
                            ins.then_inc(dsem[o.dma[0]], 16)
                        elif o.signal:
                            ins.then_inc(esem[ename], 1)
                    if ename == "sp":
                        for p in range(NPOOL):
                            if self.pool_cnt[p] > 0:
                                eng.wait_ge(dsem[p], 16 * self.pool_cnt[p])
                return body
            for ename in ENGS:
                getattr(block, engobj[ename])(make(ename))

    @staticmethod
    def _a(x):
        return x.ap if isinstance(x, V) else x

    def mm(self, out, lhsT, rhs, start=True, stop=True, **kw):
        a = self._a
        o, l, r = a(out), a(lhsT), a(rhs)
        self.op("pe", lambda e: e.matmul(o, l, r, start=start, stop=stop, **kw),
                reads=[lhsT, rhs], writes=[out])

    def transpose(self, out, in_, ident):
        a = self._a
        o, i, d = a(out), a(in_), a(ident)
        self.op("pe", lambda e: e.transpose(o, i, d), reads=[in_, ident], writes=[out])

    def act(self, out, in_, func, bias=None, scale=1.0, accum_out=None):
        a = self._a
        o, i = a(out), a(in_)
        kw = {"scale": a(scale)}
        if bias is not None:
            kw["bias"] = a(bias)
        if accum_out is not None:
            kw["accum_out"] = a(accum_out)
        self.op("act", lambda e: e.activation(o, i, func, **kw),
                reads=[in_, bias, scale], writes=[out, accum_out])

    def tt(self, out, in0, in1, op, eng="dve"):
        a = self._a
        o, x, y = a(out), a(in0), a(in1)
        self.op(eng, lambda e: e.tensor_tensor(o, x, y, op), reads=[in0, in1], writes=[out])

    def ts(self, out, in0, s1, op0, s2=None, op1=None, eng="dve"):
        a = self._a
        o, x, p, q = a(out), a(in0), a(s1), a(s2)
        kw = {}
        if op1 is not None:
            kw["op1"] = op1
        self.op(eng, lambda e: e.tensor_scalar(o, x, p, q, op0, **kw),
                reads=[in0, s1, s2], writes=[out])

    def stt(self, out, in0, scalar, in1, op0, op1, eng="dve"):
        a = self._a
        o, x, s, y = a(out), a(in0), a(scalar), a(in1)
        self.op(eng, lambda e: e.scalar_tensor_tensor(o, x, s, y, op0, op1),
                reads=[in0, scalar, in1], writes=[out])

    def copy(self, out, in_, eng="dve"):
        a = self._a
        o, i = a(out), a(in_)
        if eng == "act":
            self.op("act", lambda e: e.activation(o, i, AF.Copy), reads=[in_], writes=[out])
        else:
            self.op(eng, lambda e: e.tensor_copy(o, i), reads=[in_], writes=[out])

    def memset(self, out, val, eng="pool"):
        o = self._a(out)
        self.op(eng, lambda e: e.memset(o, val), reads=[], writes=[out])


def _bf(a):
    return np.asarray(a, dtype=np.float32).astype(ml_dtypes.bfloat16)


def host_consts():
    c = {}
    p = np.arange(128)
    c["ident"] = np.eye(128, dtype=np.float32)
    c["identb"] = _bf(np.eye(128))
    k = p[:, None]
    q = p[None, :]
    c["causneg"] = _bf(np.where(k <= q, 0.0, -BIG))
    c["strictneg"] = _bf(np.where(k < q, 0.0, -BIG))
    es = np.zeros((33, 32, 128), np.float32)
    for j in range(32):
        es[j, j, :] = BIG
    es[32, :, :] = -BIG
    c["esel"] = _bf(es)
    c["blk64"] = _bf((p[:, None] // 64 == p[None, :] // 64) / 64.0)
    c["on96"] = _bf(np.full((96, 96), 1.0 / 96))
    c["on192"] = _bf(np.full((128, 128), 1.0 / 192))
    c["on128"] = _bf(np.full((128, 128), 1.0 / 128))
    wn = np.full((65, 64), 1.0 / 64, np.float32)
    wn[64, :] = EPS
    c["wn"] = _bf(wn)
    c["negtri"] = _bf(np.where(p[:, None] >= p[None, :], -1.0, 0.0))
    c["onesb"] = _bf(np.ones((128, 128)))
    ra = np.zeros((128, 128), np.float32)
    for base in (0, 64):
        for d in range(32):
            ra[base + d + 32, base + d] = -1.0
            ra[base + d, base + d + 32] = 1.0
    c["rotA"] = _bf(ra)
    rm = np.zeros((96, 96), np.float32)
    for j in range(16):
        rm[80 + j, 64 + j] = -1.0
        rm[64 + j, 80 + j] = 1.0
    c["rotM"] = _bf(rm)
    c["triu"] = np.where(p[:, None] <= p[None, :], 1.0, 0.0).astype(np.float32)
    c["negmaskT"] = np.where(p[:, None] <= p[None, :], 0.0, -30000.0).astype(np.float32)
    pn = np.zeros((128, 8, 4, 8), np.float32)
    for cur in range(8):
        pn[:, cur, :, cur:] = -3.0e38
    c["pastneg"] = pn
    inv_a = (1.0 / (np.float32(THETA) ** (np.arange(0, 64, 2, dtype=np.float32) / np.float32(64)))).astype(np.float32)
    inv_m = (1.0 / (np.float32(THETA) ** (np.arange(0, 32, 2, dtype=np.float32) / np.float32(32)))).astype(np.float32)
    iv = np.zeros((128, 2), np.float32)
    iv[:, 0] = (inv_a[p % 32].astype(np.float64) / (2 * np.pi)).astype(np.float32)
    iv[64:96, 1] = (inv_m[(p[64:96] - 64) % 16].astype(np.float64) / (2 * np.pi)).astype(np.float32)
    c["invf"] = iv
    c["onesrow"] = _bf(np.ones((1, 128)))
    c["ones32"] = np.ones((128, 128), np.float32)
    return c


WNAMES = ["mix_norm_g", "w_in", "moba_qk_g", "mla_q_norm_g", "mla_kv_norm_g", "mla_w_uq", "mla_w_ukv",
          "mla_qk_g", "ssm_conv_w", "ssm_conv_b", "ssm_dt_bias", "ssm_a_log", "ssm_d", "ssm_norm_g",
          "head_out_g", "w_out", "ffn_norm_g", "ffn_w_in", "ffn_conv_w", "ffn_conv_b", "ffn_w_out"]
WSHAPES = {"mix_norm_g": (2, 1024), "w_in": (2, 1024, 2660), "moba_qk_g": (2, 2, 64), "mla_q_norm_g": (2, 192),
           "mla_kv_norm_g": (2, 128), "mla_w_uq": (2, 192, 384), "mla_w_ukv": (2, 128, 512), "mla_qk_g": (2, 2, 96),
           "ssm_conv_w": (2, 4, 512), "ssm_conv_b": (2, 512), "ssm_dt_bias": (2, 4), "ssm_a_log": (2, 4),
           "ssm_d": (2, 4), "ssm_norm_g": (2, 256), "head_out_g": (2, 3, 4, 64), "w_out": (2, 1024, 1024),
           "ffn_norm_g": (2, 1024), "ffn_w_in": (2, 1024, 5632), "ffn_conv_w": (2, 3, 5632),
           "ffn_conv_b": (2, 5632), "ffn_w_out": (2, 2816, 1024)}

SEQ = 2048
DM = 1024
NT = 16
NQB = 4


def build(NL=2, NS=2, dbg=False, phases=("moba", "mla", "sb", "ssd", "ffn")):
    nc = bass.Bass("TRN2", target_bir_lowering=False)
    S = Sched(nc)
    CONST = S.tile("const", const=True)

    def din(name, shape, dt=F32):
        return nc.dram_tensor(name, list(shape), dt, kind="ExternalInput").ap()

    x_d = din("x", [NS, SEQ, DM])
    pos_d = din("positions", [NS, SEQ], I32)
    W = {n: din(n, WSHAPES[n]) for n in WNAMES}
    hc = host_consts()
    CD = {n: din("c_" + n, a.shape, BF16 if a.dtype == ml_dtypes.bfloat16 else F32) for n, a in hc.items()}
    out_d = nc.dram_tensor("out", [NS, SEQ, DM], F32, kind="ExternalOutput").ap()
    out_t = [[S.tile(f"out{s}_{t}") for t in range(NT)] for s in range(NS)]
    dbg_out = {}

    def dbgdump(name, v, shape):
        if not dbg:
            return
        d = nc.dram_tensor("dbg_" + name, list(shape), v.ap.dtype, kind="ExternalOutput").ap()
        dbg_out[name] = d
        S.dma(V(d, S.tile()), v)

    cnt = [0]

    def sb(shape, dt=F32, name=None):
        cnt[0] += 1
        return nc.alloc_sbuf_tensor(name or f"sb{cnt[0]}", list(shape), dt).ap()

    def sbv(shape, dt=F32, name=None, const=False):
        return V(sb(shape, dt, name), S.tile(name, const=const))

    PS = [V(nc.alloc_psum_tensor(f"ps{i}", [128, 512], F32).ap(), S.tile(f"ps{i}")) for i in range(8)]
    ps_rr = {}

    def psb(role):
        banks = {"A": (0, 1), "B": (2, 3), "C": (4, 5), "D": (6, 7)}[role]
        i = ps_rr.get(role, 0)
        ps_rr[role] = i + 1
        return PS[banks[i % len(banks)]]

    C = {}
    for n, a in hc.items():
        v = sbv(a.shape, BF16 if a.dtype == ml_dtypes.bfloat16 else F32, name="k_" + n)
        S.dma(v, V(CD[n], CONST))
        v.t.const = False
        C[n] = v
    ident, identb = C["ident"], C["identb"]

    WB16 = {}

    def cast_weight(name, l, rows, cols):
        dst = nc.dram_tensor(f"{name}_b16_{l}", [rows, cols], BF16, kind="Internal").ap()
        src = W[name][l]
        tiles = []
        for r0 in range(0, rows, 512):
            for c0 in range(0, cols, 2048):
                r1, c1 = min(rows, r0 + 512), min(cols, c0 + 2048)
                t = S.tile()
                S.dma(V(dst[r0:r1, c0:c1], t), V(src[r0:r1, c0:c1], CONST), eng="pool")
                tiles.append(V(None, t))
        WB16[(name, l)] = (dst, tiles)

    for l in range(NL):
        cast_weight("w_in", l, 1024, 2660)
        cast_weight("mla_w_uq", l, 192, 384)
        cast_weight("mla_w_ukv", l, 128, 512)
        cast_weight("w_out", l, 1024, 1024)
        cast_weight("ffn_w_in", l, 1024, 5632)
        cast_weight("ffn_w_out", l, 2816, 1024)

    hT_ap = sb([128, 8, SEQ], BF16, "hT")
    hT_t = [S.tile(f"hT{b}") for b in range(NQB)]
    mixT_ap = sb([128, 8, SEQ], BF16, "mixT")
    mixT_t = [[S.tile(f"mx{c}_{b}") for b in range(NQB)] for c in range(8)]
    ARN = 21504
    arena_ap = sb([128, ARN], BF16, "arena")
    NQK = 32
    VOFF = NQK * 512
    arena_tiles = [S.tile(f"ar{i}") for i in range(NQK)] + [S.tile("arv")]

    def arena_reset(old):
        nonlocal arena_tiles
        arena_tiles = S.retile(old, NQK + 1)
    WBUF = sb([128, 8 * 1024], BF16, "wbuf")
    wbuf_t = [S.tile("wbuf")]
    rope_d = nc.dram_tensor("rope_scr", [NS * 4, 128, SEQ], BF16, kind="Internal").ap()
    rope_t = [[S.tile(f"rope{i}_{b}") for b in range(NQB)] for i in range(NS * 4)]
    selT = sbv([33, SEQ], BF16, "selT")
    xt_bufs = [sbv([128, DM], F32, f"xt{i}") for i in range(2)]
    gpp = sbv([128, 16], F32, "gpp")
    pp = sb([128, 64], F32, "pp")
    pp_t = S.tile("pp")
    small = {}

    rr = {}

    poolF = [sbv([128, 512], F32, f"poolF{i}") for i in range(7)]
    poolB = [sbv([128, 512], BF16, f"poolB{i}") for i in range(8)]

    def rot(key, shape, dt, n=2, ded=False):
        free = list(shape[1:])
        nel = int(np.prod(free))
        nbytes = nel * (2 if dt == BF16 else 4)
        pool = None
        if not ded and n >= 2:
            if dt in (F32, I32) and 256 < nbytes <= 2048:
                pool, pk = poolF, "poolF"
            elif dt == BF16 and 128 < nbytes <= 1024:
                pool, pk = poolB, "poolB"
        if pool is None:
            if key not in small:
                small[key] = [sbv(shape, dt) for _ in range(n)]
            i = rr.get(key, 0)
            rr[key] = i + 1
            return small[key][i % n]
        i = rr.get(pk, 0)
        rr[pk] = i + 1
        b = pool[i % len(pool)]
        ap = b.ap
        if dt == I32:
            ap = ap.bitcast(I32)
        ap = ap[0:shape[0], 0:nel]
        if len(free) == 2:
            ap = ap.rearrange("p (a b) -> p a b", a=free[0])
        return V(ap, b.t)

    def hTv(k, c0, c1):
        return V(hT_ap[:, k, c0:c1], hT_t[c0 // 512])

    def mixv(c, p0, p1, b):
        return V(mixT_ap[p0:p1, c, b * 512:(b + 1) * 512], mixT_t[c][b])

    def arv(off, n, parts=128, p0=0):
        assert off // 512 == (off + n - 1) // 512 and off + n <= VOFF, (off, n)
        return V(arena_ap[p0:p0 + parts, off:off + n], arena_tiles[off // 512])

    def arvv():
        return arena_tiles[NQK]

    def rstd_from(out, in_, scale, eps=EPS):
        S.act(out, in_, AF.Ln, bias=EPSC[eps][0:out.ap.shape[0]], scale=scale)
        S.act(out, out, AF.Exp, scale=-0.5)

    def silu_to(out, in_, bias=None):
        p0 = 0
        shp = list(in_.ap.shape)
        nel = int(np.prod(shp[1:]))
        vb = rot("siluv", [128, 512], F32, 1, ded=True)
        eb = rot("silue", [128, 512], F32, 1, ded=True)
        pbase = getattr(in_.ap, "base_partition", lambda: 0)()
        v = vb[pbase:pbase + shp[0], 0:nel]
        e = eb[pbase:pbase + shp[0], 0:nel]
        if len(shp) == 3:
            v = V(v.ap.rearrange("p (a b) -> p a b", a=shp[1]), v.t)
            e = V(e.ap.rearrange("p (a b) -> p a b", a=shp[1]), e.t)
        if bias is not None:
            S.ts(v, in_, bias, ALU.add)
        else:
            S.copy(v, in_)
        S.act(e, v, AF.Exp, scale=-1.0)
        S.ts(e, e, 1.0, ALU.add)
        S.op("dve", lambda en, o=e.ap: en.reciprocal(o, o), reads=[e], writes=[e])
        S.tt(out, v, e, ALU.mult)

    epsc = sbv([128, 2], F32, "epsc")
    S.memset(epsc[:, 0:1], EPS)
    S.memset(epsc[:, 1:2], 1.0)
    EPSC = {EPS: epsc[:, 0:1]}
    ONE_AP = epsc[:, 1:2]

    def load_w_cols(name, l, c0, ncols, rows=1024):
        nonlocal wbuf_t
        dst, tiles = WB16[(name, l)]
        k = rows // 128
        wbuf_t = S.retile(wbuf_t, 1)
        view = WBUF[:, 0:k * ncols].rearrange("p (k c) -> p k c", k=k)
        S.dma(V(view, wbuf_t[0]), V(dst.rearrange("(k p) c -> p k c", p=128)[:, :, c0:c0 + ncols], CONST),
              extra_reads=tiles)
        return V(view, wbuf_t[0])

    def layer_params(l):
        P = {}
        ppv = V(pp, pp_t)
        col = [0]

        def newcol(n=1):
            c0 = col[0]
            col[0] += n
            return c0

        def ld(dst_rows, c, src_ap):
            S.dma(V(pp[dst_rows[0]:dst_rows[1], c:c + 1], pp_t), V(src_ap, CONST))

        for i, nm in enumerate(("moba_gq", "moba_gk")):
            c = newcol()
            src = W["moba_qk_g"][l, i].rearrange("(d o) -> d o", o=1)
            ld((0, 64), c, src)
            ld((64, 128), c, src)
            P[nm] = c
        c = newcol()
        S.ts(ppv[:, c:c + 1], ppv[:, P["moba_gq"]:P["moba_gq"] + 1], 0.125, ALU.mult)
        P["moba_gq_s"] = c
        g = W["mla_q_norm_g"][l].rearrange("(d o) -> d o", o=1)
        c = newcol(); ld((0, 128), c, g[0:128]); P["mla_gqA"] = c
        c = newcol(); ld((0, 64), c, g[128:192]); P["mla_gqB"] = c
        c = newcol(); ld((0, 128), c, W["mla_kv_norm_g"][l].rearrange("(d o) -> d o", o=1)); P["mla_gkv"] = c
        c = newcol(); ld((0, 96), c, W["mla_qk_g"][l, 0].rearrange("(d o) -> d o", o=1)); P["mla_qg"] = c
        c2 = newcol()
        S.ts(ppv[0:96, c2:c2 + 1], ppv[0:96, c:c + 1], float(96 ** -0.5), ALU.mult)
        P["mla_qg_s"] = c2
        c = newcol(); ld((0, 96), c, W["mla_qk_g"][l, 1].rearrange("(d o) -> d o", o=1)); P["mla_kg"] = c
        c = newcol(12)
        S.dma(V(pp[0:64, c:c + 12], pp_t), V(W["head_out_g"][l].rearrange("m h d -> d (m h)"), CONST),
              allow_slow_non_contiguous=True)
        P["hog"] = c
        c = newcol(16)
        for tap in range(4):
            S.dma(V(pp[:, c + tap * 4:c + tap * 4 + 4], pp_t),
                  V(W["ssm_conv_w"][l, tap].rearrange("(c p) -> p c", p=128), CONST), allow_slow_non_contiguous=True)
        P["cw"] = c
        c = newcol(4)
        S.dma(V(pp[:, c:c + 4], pp_t), V(W["ssm_conv_b"][l].rearrange("(c p) -> p c", p=128), CONST),
              allow_slow_non_contiguous=True)
        P["cb"] = c
        for nm, key in (("dtb", "ssm_dt_bias"), ("alog", "ssm_a_log"), ("dsk", "ssm_d")):
            c = newcol(4)
            S.dma(V(pp[:, c:c + 4], pp_t),
                  V(W[key][l].rearrange("(o h) -> o h", o=1).to_broadcast([128, 4]), CONST))
            P[nm] = c
        c = newcol(4)
        S.act(ppv[:, c:c + 4], ppv[:, P["alog"]:P["alog"] + 4], AF.Exp)
        S.ts(ppv[:, c:c + 4], ppv[:, c:c + 4], -1.0, ALU.mult)
        P["ahead"] = c
        assert col[0] <= 64
        return P

    def rope_tables(s):
        for b in range(NQB):
            posi = rot("posi", [128, 512], I32)
            S.dma(posi, V(pos_d[s:s + 1, b * 512:(b + 1) * 512].to_broadcast([128, 512]), CONST))
            posf = rot("posf", [128, 512], F32, 1, ded=True)
            S.copy(posf, posi)
            for ti, (col, phase) in enumerate(((0, 0.25), (0, 0.0), (1, 0.25), (1, 0.0))):
                ang = rot("ang", [128, 512], F32)
                S.ts(ang, posf, C["invf"][:, col:col + 1], ALU.mult, float(phase), ALU.add)
                ai = rot("angi", [128, 512], I32)
                S.copy(ai, ang)
                af = rot("angf", [128, 512], F32)
                S.copy(af, ai)
                fr = rot("frac", [128, 512], F32)
                S.tt(fr, ang, af, ALU.subtract)
                m1 = rot("frm", [128, 512], F32)
                S.ts(m1, fr, 0.5, ALU.is_gt)
                S.tt(fr, fr, m1, ALU.subtract)
                m2 = rot("frm", [128, 512], F32)
                S.ts(m2, fr, -0.5, ALU.is_lt)
                S.tt(fr, fr, m2, ALU.add)
                S.ts(fr, fr, 0.4999999, ALU.min, -0.4999999, ALU.max)
                tab = rot("tab", [128, 512], BF16)
                S.act(tab, fr, AF.Sin, scale=float(2 * np.pi))
                S.dma(V(rope_d[s * 4 + ti, :, b * 512:(b + 1) * 512], rope_t[s * 4 + ti][b]), tab)

    cur_seq = [0]

    def rope_blk(ti, b, parts=128):
        v = rot("ropeb%d" % (ti % 2), [128, 512], BF16, 2, ded=True)
        tj = cur_seq[0] * 4 + ti
        S.dma(v[0:parts], V(rope_d[tj, 0:parts, b * 512:(b + 1) * 512], rope_t[tj][b]))
        return v[0:parts]

    def norm_tile_to_hT(xt, gi, tt):
        ss2 = rot("ss", [128, 2], F32, 4)
        for half in range(2):
            S.act(rot("jk", [128, 512], BF16), xt[:, half * 512:(half + 1) * 512], AF.Square,
                  accum_out=ss2[:, half:half + 1])
        ss = rot("ssum", [128, 1], F32, 4)
        S.tt(ss, ss2[:, 0:1], ss2[:, 1:2], ALU.add)
        rs = rot("rs", [128, 1], F32, 4)
        rstd_from(rs, ss, 1.0 / DM)
        for half in range(2):
            hn = rot("hn", [128, 512], F32)
            S.ts(hn, xt[:, half * 512:(half + 1) * 512], rs, ALU.mult)
            pb = psb("A")
            for j in range(4):
                S.transpose(pb[:, j * 128:(j + 1) * 128], hn[:, j * 128:(j + 1) * 128], ident)
            dst = V(hT_ap[:, half * 4:(half + 1) * 4, tt * 128:(tt + 1) * 128], hT_t[tt // 4])
            src = V(pb.ap.rearrange("p (j t) -> p j t", j=4), pb.t)
            c0 = gi * 8 + half * 4
            S.tt(dst, src, V(gpp.ap[:, c0:c0 + 4].unsqueeze(2).to_broadcast([128, 4, 128]), gpp.t), ALU.mult)

    def load_x_tile(src_ap, tile_obj):
        xt = xt_bufs[rr.get("xt", 0) % 2]
        rr["xt"] = rr.get("xt", 0) + 1
        S.dma(xt, V(src_ap, tile_obj))
        return xt

    def proj_fm(wv, c0, M, b, out_ps):
        for k in range(8):
            S.mm(out_ps[0:M, :], wv[:, k, c0:c0 + M], hTv(k, b * 512, (b + 1) * 512), start=(k == 0), stop=(k == 7))

    def proj_tm(wv, c0, N, tt, out_ps_cols):
        for k in range(8):
            S.mm(out_ps_cols, hTv(k, tt * 128, (tt + 1) * 128), wv[:, k, c0:c0 + N], start=(k == 0), stop=(k == 7))

    def head_norm_store(po, has_r, hog_col, c, h, b):
        sq = rot("hsq", [65, 512], BF16)
        nr = 65 if has_r else 64
        S.act(sq[0:nr], po[0:nr], AF.Square)
        pm = psb("C")
        S.mm(pm[0:64, :], C["wn"][0:nr, :], sq[0:nr])
        rs = rot("hrs", [64, 512], F32)
        if has_r:
            S.act(rs, pm[0:64, :], AF.Ln)
        else:
            S.act(rs, pm[0:64, :], AF.Ln, bias=EPSC[EPS][0:64])
        S.act(rs, rs, AF.Exp, scale=-0.5)
        p0 = (h % 2) * 64
        S.stt(mixv(c, p0, p0 + 64, b), po[0:64, :], V(pp[0:64, hog_col:hog_col + 1], pp_t), rs, ALU.mult, ALU.mult)

    vaug_ap = arena_ap[:, VOFF:VOFF + 16 * 4 * 66].rearrange("p (t h d) -> p t h d", t=16, h=4)

    def vaug(tt, h, n=65):
        return V(vaug_ap[:, tt, h, 0:n], arvv())

    def softmax_attention(kfn, qfn, mask_fn, hog_col, cbase):
        for h in range(4):
            c = h // 2
            for b in range(NQB):
                po = psb("D")
                nk = 4 * b + 4
                pend = None
                for i in range(nk):
                    r = i - 4 * b
                    psx = psb("A")
                    q0, extra = mask_fn(psx, h, b, i, r)
                    S.mm(psx[:, q0:512], kfn(h, i), qfn(h, b, q0), start=True, stop=(len(extra) == 0))
                    for ei, (eo, el, er) in enumerate(extra):
                        S.mm(eo, el, er, start=False, stop=(ei == len(extra) - 1))
                    pT = rot("pT", [128, 512], BF16, 3)
                    S.act(pT[:, q0:512], psx[:, q0:512], AF.Exp)
                    if pend is not None:
                        S.mm(*pend[0], **pend[1])
                    pend = ((po[0:65, q0:512], vaug(i, h), pT[:, q0:512]), dict(start=(i == 0), stop=(i == nk - 1)))
                S.mm(*pend[0], **pend[1])
                head_norm_store(po, True, hog_col + h, cbase + c, h, b)

    kmT = sbv([128, 4, 8], BF16, "kmT")

    def moba(l, s, P):
        wv = load_w_cols("w_in", l, 0, 768)
        qT = lambda c, b: arv(c * 2048 + b * 512, 512)
        kpad = lambda h, c0, n: arv(4096 + h * 2048 + c0, n)
        for i in range(8, 24):
            S.memset(V(arena_ap[:, i * 512:(i + 1) * 512], arena_tiles[i]), 0.0)
        S.memset(V(arena_ap[:, VOFF:VOFF + 16 * 4 * 66], arvv()), 1.0)
        S.memset(kmT, 0.0)
        for b in range(NQB):
            cosb = rope_blk(0, b)
            sinb = rope_blk(1, b)
            for which in range(2):
                gcol = P["moba_gq_s"] if which == 0 else P["moba_gk"]
                for c in range(2):
                    pq = psb("B")
                    proj_fm(wv, which * 256 + c * 128, 128, b, pq)
                    sq = rot("msq", [128, 512], BF16)
                    S.act(sq, pq, AF.Square)
                    pm = psb("C")
                    S.mm(pm, C["blk64"], sq)
                    rs = rot("mrs", [128, 512], F32)
                    rstd_from(rs, pm, 1.0)
                    qn = rot("mqn", [128, 512], BF16)
                    S.stt(qn, pq, V(pp[:, gcol:gcol + 1], pp_t), rs, ALU.mult, ALU.mult)
                    pr = psb("C")
                    S.mm(pr, C["rotA"], qn)
                    t1 = rot("mt1", [128, 512], F32)
                    S.tt(t1, qn, cosb, ALU.mult, eng="pool")
                    t2 = rot("mt2", [128, 512], F32)
                    S.tt(t2, pr, sinb, ALU.mult)
                    if which == 0:
                        S.tt(qT(c, b), t1, t2, ALU.add)
                    else:
                        ksum = rot("mks", [128, 512], F32)
                        S.tt(ksum, t1, t2, ALU.add)
                        for hh in range(2):
                            h = 2 * c + hh
                            S.copy(kpad(h, b * 512, 512)[hh * 64:(hh + 1) * 64], ksum[hh * 64:(hh + 1) * 64],
                                   eng=("act" if hh else "pool"))
                        km = rot("mkm", [128, 2], F32)
                        S.op("dve", lambda e, o=km.ap, i=ksum.ap: e.reduce_sum(
                            o, i.rearrange("p (n j) -> p n j", n=2), AX.X), reads=[ksum], writes=[km])
                        for hh in range(2):
                            h = 2 * c + hh
                            S.ts(kmT[hh * 64:(hh + 1) * 64, h, 2 * b:2 * b + 2], km[hh * 64:(hh + 1) * 64],
                                 1.0 / 256, ALU.mult)
        for tt in range(NT):
            pv = psb("B")
            proj_tm(wv, 512, 256, tt, pv[:, 0:256])
            S.copy(V(vaug_ap[:, tt, :, 0:64], arvv()),
                   V(pv.ap[:, 0:256].rearrange("p (h d) -> p h d", h=4), pv.t), eng="act")
        S.memset(selT[32:33, :], 1.0)
        for qt in range(NT):
            pg = psb("C")
            for h in range(4):
                c = h // 2
                S.mm(pg[:, h * 8:(h + 1) * 8], qT(c, qt // 4)[:, (qt % 4) * 128:(qt % 4 + 1) * 128], kmT[:, h, :])
            gm = rot("gm", [128, 32], F32)
            S.tt(gm, pg[:, 0:32], V(C["pastneg"].ap[:, qt // 2].rearrange("p h n -> p (h n)"), C["pastneg"].t), ALU.add)
            m8 = rot("m8", [128, 4, 8], F32)
            for h in range(4):
                S.op("dve", lambda e, o=m8.ap[:, h, :], i=gm.ap[:, h * 8:(h + 1) * 8]: e.max(o, i),
                     reads=[gm], writes=[m8])
            thr = rot("thr", [128, 4], F32)
            S.ts(thr, V(m8.ap[:, :, 2], m8.t), -1.0e30, ALU.max)
            sel = rot("sel", [128, 32], F32)
            S.tt(V(sel.ap.rearrange("p (h n) -> p h n", h=4), sel.t), V(gm.ap.rearrange("p (h n) -> p h n", h=4), gm.t),
                 V(thr.ap.unsqueeze(2).to_broadcast([128, 4, 8]), thr.t), ALU.is_ge)
            pt = psb("C")
            S.transpose(pt[0:32, 0:128], sel, ident)
            S.copy(selT[0:32, qt * 128:(qt + 1) * 128], pt[0:32, 0:128], eng="act")

        if dbg and s == 0 and l == 0:
            allar = S.retile(arena_tiles, 1)[0]
            dbgdump("moba_arena", V(arena_ap, allar), [128, ARN])
            arena_reset([allar])
            dbgdump("moba_selT", selT, [33, SEQ])
            dbgdump("moba_kmT", kmT, [128, 4, 8])

        def mask_fn(psx, h, b, i, r):
            q0 = 0 if r < 0 else (128 * r if r < 2 else max(256, 128 * r))
            n = i // 2
            if r < 0:
                return q0, [(psx, C["esel"][:, h * 8 + n, :], selT[:, b * 512:(b + 1) * 512])]
            ex = []
            if r < 2:
                ex.append((psx[:, 256:512], C["esel"][:, h * 8 + n, :], selT[:, b * 512 + 256:(b + 1) * 512]))
            ex.append((psx[:, 128 * r:128 * (r + 1)], identb, C["causneg"]))
            return q0, ex

        softmax_attention(lambda h, i: kpad(h, i * 128, 128), lambda h, b, q0: qT(h // 2, b)[:, q0:512],
                          mask_fn, P["hog"] + 0, 0)

    wuq = sbv([128, 2, 384], BF16, "wuq")
    wkv = sbv([128, 512], BF16, "wkv")

    def mla(l, s, P):
        wv = load_w_cols("w_in", l, 768, 352)
        dq, tq = WB16[("mla_w_uq", l)]
        S.dma(wuq[:, 0, :], V(dq[0:128, :], CONST), extra_reads=tq)
        S.dma(wuq[0:64, 1, :], V(dq[128:192, :], CONST), extra_reads=tq)
        dk, tk = WB16[("mla_w_ukv", l)]
        S.dma(wkv, V(dk, CONST), extra_reads=tk)
        qf = lambda h, c0, n: arv(h * 2048 + c0, n, parts=96)
        kf = lambda h, c0, n: arv(8192 + h * 2048 + c0, n, parts=96)
        S.memset(V(arena_ap[:, VOFF:VOFF + 16 * 4 * 66], arvv()), 1.0)
        ppc = lambda col, p0, p1: V(pp[p0:p1, col:col + 1], pp_t)

        def qk_finish(pre, gcol, dstv, cosb, sinb):
            sq = rot("lsq", [96, 512], BF16)
            S.act(sq, pre, AF.Square)
            pm = psb("C")
            S.mm(pm[0:96, :], C["on96"], sq)
            rs = rot("lrs", [96, 512], F32)
            rstd_from(rs, pm[0:96, :], 1.0)
            qn = rot("lqn", [96, 512], BF16)
            S.stt(qn, pre, ppc(gcol, 0, 96), rs, ALU.mult, ALU.mult)
            pr = psb("C")
            S.mm(pr[0:96, :], C["rotM"], qn)
            t1 = rot("lt1", [96, 512], F32)
            S.tt(t1, qn, cosb, ALU.mult, eng="pool")
            t2 = rot("lt2", [96, 512], F32)
            S.tt(t2, pr[0:96, :], sinb, ALU.mult)
            S.tt(dstv, t1, t2, ALU.add)

        for b in range(NQB):
            cosb = rope_blk(2, b, 96)
            sinb = rope_blk(3, b, 96)
            pA = psb("B"); proj_fm(wv, 0, 128, b, pA)
            pB = psb("B"); proj_fm(wv, 128, 64, b, pB)
            sqA = rot("lsqA", [128, 512], BF16)
            sqB = rot("lsqB", [64, 512], BF16)
            S.act(sqA, pA, AF.Square)
            S.act(sqB, pB[0:64, :], AF.Square)
            pm = psb("C")
            S.mm(pm, C["on192"], sqA, start=True, stop=False)
            S.mm(pm, C["on192"][0:64, :], sqB, start=False, stop=True)
            rs = rot("lrsq", [128, 512], F32)
            rstd_from(rs, pm, 1.0)
            cqA = rot("cqA", [128, 512], BF16, 1)
            cqB = rot("cqB", [64, 512], BF16, 1)
            S.stt(cqA, pA, ppc(P["mla_gqA"], 0, 128), rs, ALU.mult, ALU.mult)
            S.stt(cqB, pB[0:64, :], ppc(P["mla_gqB"], 0, 64), rs[0:64], ALU.mult, ALU.mult)
            pC = psb("B"); proj_fm(wv, 192, 128, b, pC)
            sqC = rot("lsqA", [128, 512], BF16)
            S.act(sqC, pC, AF.Square)
            pm2 = psb("C")
            S.mm(pm2, C["on128"], sqC)
            rs2 = rot("lrsq", [128, 512], F32)
            rstd_from(rs2, pm2, 1.0)
            ckv = rot("ckv", [128, 512], BF16, 1)
            S.stt(ckv, pC, ppc(P["mla_gkv"], 0, 128), rs2, ALU.mult, ALU.mult)
            pD = psb("B"); proj_fm(wv, 320, 32, b, pD)
            kpre = rot("kpre", [96, 512], F32, 1)
            S.copy(kpre[64:96, :], pD[0:32, :], eng="act")
            for h in range(4):
                pq = psb("B")
                S.mm(pq[0:96, :], wuq[:, 0, h * 96:(h + 1) * 96], cqA, start=True, stop=False)
                S.mm(pq[0:96, :], wuq[0:64, 1, h * 96:(h + 1) * 96], cqB, start=False, stop=True)
                qk_finish(pq[0:96, :], P["mla_qg_s"], qf(h, b * 512, 512), cosb, sinb)
                pk = psb("B")
                S.mm(pk[0:64, :], wkv[:, h * 128:h * 128 + 64], ckv)
                S.copy(kpre[0:64, :], pk[0:64, :], eng="act")
                qk_finish(kpre, P["mla_kg"], kf(h, b * 512, 512), cosb, sinb)
            for j in range(4):
                tt = 4 * b + j
                pv = psb("B")
                S.mm(pv[:, 0:256], ckv[:, j * 128:(j + 1) * 128],
                     V(wkv.ap.rearrange("p (h t d) -> p h t d", h=4, t=2)[:, :, 1, :], wkv.t))
                S.copy(V(vaug_ap[:, tt, :, 0:64], arvv()),
                       V(pv.ap[:, 0:256].rearrange("p (h d) -> p h d", h=4), pv.t), eng="act")

        def mask_fn(psx, h, b, i, r):
            q0 = 0 if r < 0 else 128 * r
            if r < 0:
                return q0, []
            return q0, [(psx[:, q0:q0 + 128], identb, C["causneg"])]

        if dbg and s == 0 and l == 0:
            allar = S.retile(arena_tiles, 1)[0]
            dbgdump("mla_arena", V(arena_ap, allar), [128, ARN])
            arena_reset([allar])
        softmax_attention(lambda h, i: kf(h, i * 128, 128), lambda h, b, q0: qf(h, b * 512, 512)[:, q0:512],
                          mask_fn, P["hog"] + 4, 2)

    def sbmix(l, s, P):
        wv = load_w_cols("w_in", l, 1120, 768)
        qT = lambda c, b: arv(c * 2048 + b * 512, 512)
        kpad = lambda h, c0, n: arv(4096 + h * 2048 + c0, n)
        vs_ap = arena_ap[:, VOFF:VOFF + 16 * 256].rearrange("p (t h d) -> p t h d", t=16, h=4)
        vsv = lambda tt, h: V(vs_ap[:, tt, h, :], arvv())
        for i in range(8, 24):
            S.memset(V(arena_ap[:, i * 512:(i + 1) * 512], arena_tiles[i]), 0.0)
        for c in range(2):
            for b in range(NQB):
                pq = psb("B")
                proj_fm(wv, c * 128, 128, b, pq)
                S.act(qT(c, b), pq, AF.Copy, scale=0.125)
                pk = psb("B")
                proj_fm(wv, 256 + c * 128, 128, b, pk)
                for hh in range(2):
                    h = 2 * c + hh
                    S.copy(kpad(h, b * 512, 512)[hh * 64:(hh + 1) * 64], pk[hh * 64:(hh + 1) * 64, :],
                           eng=("dve" if hh == 0 else "act"))
        for tt in range(NT):
            pv = psb("B")
            proj_tm(wv, 512, 256, tt, pv[:, 0:256])
            S.copy(V(vs_ap[:, tt, :, :], arvv()), V(pv.ap[:, 0:256].rearrange("p (h d) -> p h d", h=4), pv.t),
                   eng="act")
        for h in range(4):
            c = h // 2
            for b in range(NQB):
                po = psb("D")
                nk = 4 * b + 4
                Rs = rot("sbR", [128, 512], F32, 2, ded=True)
                first = True
                for i in range(nk - 1, -1, -1):
                    r = i - 4 * b
                    q0 = 0 if r < 0 else 128 * r
                    w = slice(q0, 512)
                    psx = psb("A")
                    S.mm(psx[:, w], kpad(h, i * 128, 128), qT(c, b)[:, w], start=True, stop=False)
                    if r >= 0:
                        S.mm(psx[:, q0:q0 + 128], identb, C["strictneg"], start=False, stop=False)
                    e = rot("sbe", [128, 512], F32, 2)
                    S.act(e[:, w], psx[:, w], AF.Exp)
                    sp = rot("sbsp", [128, 512], BF16, 2)
                    S.act(sp[:, w], e[:, w], AF.Ln, bias=ONE_AP)
                    S.mm(psx[:, w], C["negtri"], sp[:, w], start=False, stop=True, skip_group_check=True)
                    a = rot("sba", [128, 512], BF16, 3)
                    if first:
                        S.act(a[:, w], psx[:, w], AF.Exp)
                    else:
                        d = rot("sbd", [128, 512], F32, 2)
                        S.tt(d[:, w], psx[:, w], Rs[:, w], ALU.subtract)
                        S.act(a[:, w], d[:, w], AF.Exp)
                    if i > 0:
                        pt = psb("C")
                        S.mm(pt[:, w], C["onesb"], sp[:, w])
                        if first:
                            if q0 > 0:
                                S.memset(Rs[:, 0:q0], 0.0, eng="pool")
                            S.copy(Rs[:, w], pt[:, w])
                        else:
                            S.tt(Rs[:, w], Rs[:, w], pt[:, w], ALU.add)
                    S.mm(po[0:64, w], vsv(i, h), a[:, w], start=(i == nk - 1), stop=(i == 0),
                         skip_group_check=True)
                    first = False
                head_norm_store(po, False, P["hog"] + 8 + h, 4 + c, h, b)

    diagw = sbv([128, 16, 128], BF16, "diagw")
    diagD = sbv([128, 4, 128], BF16, "diagD")

    def ssd(l, s, P):
        wv = load_w_cols("w_in", l, 1888, 772)
        ppc = lambda col, n=1: V(pp[:, col:col + n], pp_t)
        XW = 2056
        ssd_t = S.retile(arena_tiles, 3)
        xbc = lambda cc, c0, n: V(arena_ap[:, cc * XW + c0: cc * XW + c0 + n], ssd_t[0])
        BT0 = 4 * XW
        btpad = lambda g, c0, n: V(arena_ap[:, BT0 + g * 2048 + c0: BT0 + g * 2048 + c0 + n], ssd_t[1])
        CT0 = BT0 + 4096
        ctpad = lambda g, c0, n: V(arena_ap[:, CT0 + g * 2048 + c0: CT0 + g * 2048 + c0 + n], ssd_t[2])
        assert CT0 + 4096 <= ARN
        S.memset(V(arena_ap[:, 0:BT0], ssd_t[0]), 0.0)
        S.memset(V(arena_ap[:, BT0:CT0], ssd_t[1]), 0.0)
        S.memset(V(arena_ap[:, CT0:CT0 + 4096], ssd_t[2]), 0.0)
        for tap in range(4):
            for cc in range(4):
                S.ts(diagw[:, tap * 4 + cc, :], identb, ppc(P["cw"] + tap * 4 + cc), ALU.mult, eng="pool")
        for h in range(4):
            S.ts(diagD[:, h, :], identb, ppc(P["dsk"] + h), ALU.mult, eng="pool")
        gn = rot("ssm_gn", [128, 256], F32, 1)
        S.dma(gn, V(W["ssm_norm_g"][l].rearrange("(o c) -> o c", o=1).to_broadcast([128, 256]), CONST))
        for cc in range(4):
            for b in range(NQB):
                px = psb("B")
                proj_fm(wv, 256 + cc * 128, 128, b, px)
                S.copy(xbc(cc, 3 + b * 512, 512), px, eng=("act" if (cc + b) % 2 else "dve"))
        for cc in (2, 3):
            for b in range(NQB):
                pc = psb("B")
                for tap in range(4):
                    S.mm(pc, diagw[:, tap * 4 + cc, :], xbc(cc, b * 512 + tap, 512), start=(tap == 0), stop=(tap == 3))
                for g in range(2):
                    dst = (btpad if cc == 2 else ctpad)(g, b * 512, 512)
                    silu_to(dst[g * 64:(g + 1) * 64], pc[g * 64:(g + 1) * 64, :],
                            bias=V(pp[g * 64:(g + 1) * 64, P["cb"] + cc:P["cb"] + cc + 1], pp_t))
        import os
        stop = int(os.environ.get("SSD_STOP", "99"))
        if stop <= 1:
            arena_reset(ssd_t); return
        hst = rot("hst", [128, 4, 64], F32, 1)
        S.memset(hst, 0.0)
        Bp = rot("Bp", [128, 2, 128], BF16, 1)
        S.memset(Bp, 0.0)
        for tt in range(NT):
            t0 = tt * 128
            pz = psb("B")
            proj_tm(wv, 0, 256, tt, pz[:, 0:256])
            if not os.environ.get("SSD_NODT"):
                proj_tm(wv, 768, 4, tt, pz[:, 256:260])
            else:
                proj_tm(wv, 760, 12, tt, pz[:, 248:260])
            sub = int(os.environ.get("SSD_SUB", "99"))
            if sub <= 0:
                continue
            zs = rot("zs", [128, 256], F32, 1, ded=True)
            silu_to(zs, pz[:, 0:256])
            if sub <= 1:
                continue
            dtv = rot("dtv", [128, 4], F32)
            S.tt(dtv, pz[:, 256:260], ppc(P["dtb"], 4), ALU.add)
            S.act(dtv, dtv, AF.Exp)
            S.act(dtv, dtv, AF.Ln, bias=ONE_AP)
            av = rot("av", [128, 4], F32)
            S.tt(av, dtv, ppc(P["ahead"], 4), ALU.mult)
            if sub <= 2:
                continue
            if sub == 48:
                xs = rot("xs", [128, 256], BF16)
                S.memset(xs, 0.5)
            pcv = psb("C")
            for cc in range(3 if sub != 48 else 0):
                for tap in range(4):
                    S.mm(pcv[:, cc * 128:(cc + 1) * 128], diagw[:, tap * 4 + cc, :],
                         xbc(cc, t0 + (tap if sub != 43 else 2 * (tap // 2)), 128),
                         start=(tap == 0), stop=(tap == 3))
            cvs = rot("cvs", [128, 3, 128], BF16)
            for cc in range(3 if sub != 48 else 0):
                silu_to(cvs[:, cc, :], pcv[:, cc * 128:(cc + 1) * 128], bias=ppc(P["cb"] + cc))
            if sub == 50:
                xtr = rot("xtr", [128, 3, 128], BF16)
                for cc in range(3):
                    S.dma(xtr[:, cc, :], cvs[:, cc, :], transpose=True)
                ptr = psb("C")
                S.mm(ptr[:, 0:384], identb, V(xtr.ap.rearrange("p a b -> p (a b)"), xtr.t))
                continue
            if sub == 49:
                pdum = psb("C")
                for cc in range(3):
                    S.mm(pdum[:, cc * 128:(cc + 1) * 128], identb, C["causneg"])
                continue
            if sub <= 3:
                continue
            if sub == 40:
                xtmp = rot("xtmp", [128, 3, 128], BF16)
                S.copy(xtmp, cvs)
                continue
            if sub == 41 or sub == 43 or sub == 47:
                ptr = psb("C")
                S.mm(ptr[:, 0:128], cvs[:, 0, :], identb)
                continue
            if sub == 42:
                ptr = psb("A")
                for cc in range(3):
                    S.mm(ptr[:, cc * 128:(cc + 1) * 128], cvs[:, cc, :], identb)
                continue
            if sub != 48:
                ptr = psb("C")
                for cc in range(3):
                    S.mm(ptr[:, cc * 128:(cc + 1) * 128], cvs[:, cc, :], identb)
                if sub <= 4:
                    continue
                xs = rot("xs", [128, 256], BF16)
                S.copy(xs, ptr[:, 0:256], eng="act")
                for g in range(2):
                    S.copy(Bp[:, g, g * 64:(g + 1) * 64], ptr[:, 256 + g * 64:256 + (g + 1) * 64],
                           eng=("dve" if g else "act"))
            if stop <= 2:
                continue
            abc = rot("abc", [128, 4, 128], F32)
            for h in range(4):
                S.ts(abc[:, h, :], C["ones32"], av[:, h:h + 1], ALU.mult, eng="pool")
            pac = psb("C")
            for h in range(4):
                S.mm(pac[:, h * 128:(h + 1) * 128], abc[:, h, :], C["triu"])
            pcol = psb("B")
            S.mm(pcol[:, 0:4], C["triu"], av)
            acs = rot("acs", [128, 4], F32)
            S.copy(acs, pcol[:, 0:4])
            alast = rot("alast", [128, 4], F32)
            S.copy(alast, V(pac.ap.rearrange("p (h l) -> p h l", h=4)[:, :, 127], pac.t))
            segm = rot("segm", [128, 4, 128], F32)
            for h in range(4):
                S.stt(segm[:, h, :], pac[:, h * 128:(h + 1) * 128], acs[:, h:h + 1], C["negmaskT"], ALU.subtract, ALU.add)
            LT = rot("LT", [128, 4, 128], F32)
            S.act(LT, segm, AF.Exp)
            EA = rot("EA", [128, 4, 128], F32)
            S.act(EA, V(pac.ap.rearrange("p (h l) -> p h l", h=4), pac.t), AF.Exp)
            if stop <= 3:
                continue
            pG = psb("C")
            for g in range(2):
                S.mm(pG[:, g * 128:(g + 1) * 128], btpad(g, t0, 128), ctpad(g, t0, 128))
            MT = rot("MT", [128, 4, 128], BF16)
            CsT = rot("CsT", [128, 4, 128], BF16)
            for h in range(4):
                g = h // 2
                S.tt(MT[:, h, :], pG[:, g * 128:(g + 1) * 128], LT[:, h, :], ALU.mult)
                S.tt(CsT[:, h, :], ctpad(g, t0, 128), EA[:, h, :], ALU.mult, eng="pool")
            xdt = rot("xdt", [128, 4, 64], BF16)
            S.tt(xdt, V(xs.ap.rearrange("p (h d) -> p h d", h=4), xs.t),
                 V(dtv.ap.unsqueeze(2).to_broadcast([128, 4, 64]), dtv.t), ALU.mult)
            dec = rot("dec", [128, 4], F32)
            S.tt(dec, alast, acs, ALU.subtract)
            S.act(dec, dec, AF.Exp)
            xdd = rot("xdd", [128, 4, 64], BF16)
            S.tt(xdd, xdt, V(dec.ap.unsqueeze(2).to_broadcast([128, 4, 64]), dec.t), ALU.mult)
            cd = rot("cd", [128, 4], F32)
            S.act(cd, alast, AF.Exp)
            prevb = rot("prevb", [128, 4, 64], BF16)
            S.copy(prevb, hst)
            py = psb("B")
            for h in range(4):
                S.mm(py[:, h * 64:(h + 1) * 64], MT[:, h, :], xdt[:, h, :], start=True, stop=False)
                S.mm(py[:, h * 64:(h + 1) * 64], CsT[:, h, :], prevb[:, h, :], start=False, stop=False)
                S.mm(py[:, h * 64:(h + 1) * 64], diagD[:, h, :], xs[:, h * 64:(h + 1) * 64], start=False, stop=True)
            pst = psb("C")
            for h in range(4):
                S.mm(pst[:, h * 64:(h + 1) * 64], Bp[:, h // 2, :], xdd[:, h, :])
            for h in range(4):
                S.stt(hst[:, h, :], hst[:, h, :], cd[:, h:h + 1], pst[:, h * 64:(h + 1) * 64], ALU.mult, ALU.add)
            if stop <= 4:
                continue
            yg = rot("yg", [128, 256], F32)
            S.tt(yg, py[:, 0:256], zs, ALU.mult)
            ss = rot("yss", [128, 1], F32)
            ysq = rot("ysq", [128, 256], F32)
            S.tt(ysq, yg, yg, ALU.mult)
            S.op("dve", lambda e, o=ss.ap, i=ysq.ap: e.reduce_sum(o, i, AX.X), reads=[ysq], writes=[ss])
            rs = rot("yrs", [128, 1], F32)
            rstd_from(rs, ss, 1.0 / 256)
            yo = rot("yo", [128, 256], F32)
            S.stt(yo, yg, rs, gn, ALU.mult, ALU.mult)
            if stop <= 5:
                continue
            pt = psb("A")
            for j in range(2):
                S.transpose(pt[:, j * 128:(j + 1) * 128], yo[:, j * 128:(j + 1) * 128], ident)
            if stop <= 6:
                continue
            for j in range(2):
                S.copy(V(mixT_ap[:, 6 + j, t0:t0 + 128], mixT_t[6 + j][tt // 4]), pt[:, j * 128:(j + 1) * 128],
                       eng="dve")
        arena_reset(ssd_t)

    def out_proj_and_norm2(l, s, src_ap_fn, src_tile_fn, g2):
        nonlocal wbuf_t
        dst, tiles = WB16[("w_out", l)]
        wbuf_t = S.retile(wbuf_t, 1)
        wo = V(WBUF.rearrange("p (k c) -> p k c", k=8), wbuf_t[0])
        S.dma(wo, V(dst.rearrange("(k p) c -> p k c", p=128), CONST), extra_reads=tiles)
        for tt in range(NT):
            xt = load_x_tile(src_ap_fn(tt), src_tile_fn(tt))
            for half in range(2):
                pw = psb("B")
                for k in range(8):
                    S.mm(pw, V(mixT_ap[:, k, tt * 128:(tt + 1) * 128], mixT_t[k][tt // 4]),
                         wo[:, k, half * 512:(half + 1) * 512], start=(k == 0), stop=(k == 7))
                S.tt(xt[:, half * 512:(half + 1) * 512], pw, xt[:, half * 512:(half + 1) * 512], ALU.add)
            S.dma(V(out_d[s, tt * 128:(tt + 1) * 128, :], out_t[s][tt]), xt)
            norm_tile_to_hT(xt, g2, tt)

    def ffn(l, s):
        nonlocal mixT_t
        dwi, twi = WB16[("ffn_w_in", l)]
        dwo, two = WB16[("ffn_w_out", l)]
        dwi_r = dwi.rearrange("(k p) c -> p k c", p=128)
        ar_old = arena_tiles
        newt = S.retile(ar_old, 22 + 4 + 2)
        act_tiles, wo_t, ub_t = newt[0:22], newt[22:26], newt[26:28]
        actT_ap = arena_ap[:, 0:22 * 512].rearrange("p (f t) -> p f t", f=22)
        wo_bufs = [V(arena_ap[:, 11264 + i * 1024: 11264 + (i + 1) * 1024], t) for i, t in enumerate(wo_t)]
        ub_bufs = [V(arena_ap[:, 15360 + i * 516: 15360 + i * 516 + 514], t) for i, t in enumerate(ub_t)]
        mx_old = [t for row in mixT_t for t in row]
        wi_t = S.retile(mx_old, 4)
        mflat = mixT_ap.rearrange("p k t -> p (k t)")
        wi_bufs = [V(mflat[:, i * 4096:(i + 1) * 4096].rearrange("p (k c) -> p k c", k=8), t)
                   for i, t in enumerate(wi_t)]
        cwr = rot("cwr", [44, 3, 128], F32, 1)
        S.dma(cwr, V(W["ffn_conv_w"][l].rearrange("t (c p) -> c t p", p=128), CONST))
        cbr = rot("cbr", [44, 128], F32, 1)
        S.dma(cbr, V(W["ffn_conv_b"][l].rearrange("(c p) -> c p", p=128), CONST))
        cwf = rot("cwf", [128, 4, 44], F32, 1)
        for t_ in range(4):
            pt = psb("C")
            src = cwr[:, t_, :] if t_ < 3 else cbr
            S.transpose(pt[:, 0:44], src, ident[0:44, 0:44])
            S.copy(cwf[:, t_, :], pt[:, 0:44])
        halo = rot("halo", [128, 44, 2], BF16, 1)
        S.memset(halo, 0.0)
        ubi = [0]

        def conv_part(pu, fi, func):
            ub = ub_bufs[ubi[0] % 2]
            ubi[0] += 1
            dgs = []
            for tap in range(3):
                dg = rot("dgf", [128, 128], BF16, 4)
                S.ts(dg, identb, cwf[:, tap, fi:fi + 1], ALU.mult, eng="pool")
                dgs.append(dg)
            S.copy(ub[:, 0:2], halo[:, fi, :], eng="pool")
            S.copy(ub[:, 2:514], pu, eng="act")
            S.copy(halo[:, fi, :], ub[:, 512:514], eng="pool")
            pc = psb("C")
            for tap in range(3):
                S.mm(pc, dgs[tap], ub[:, tap:tap + 512], start=(tap == 0), stop=(tap == 2))
            res = rot("cres%d" % (fi // 22), [128, 512], F32, 2)
            if func == AF.Silu:
                silu_to(res, pc, bias=cwf[:, 3, fi:fi + 1])
            else:
                S.ts(res, pc, cwf[:, 3, fi:fi + 1], ALU.add)
            return res

        pend = [None]
        gres = {}

        def finish(pu, part, fc):
            r_ = conv_part(pu, part * 22 + fc, AF.Silu if part == 0 else AF.Identity)
            if part == 0:
                gres[fc] = r_
            else:
                S.tt(V(actT_ap[:, fc, :], act_tiles[fc]), gres.pop(fc), r_, ALU.mult)

        groups = [(g0, min(4, 22 - g0)) for g0 in range(0, 22, 4)]
        for b in range(NQB):
            for gi, (g0, ng) in enumerate(groups):
                wg = wi_bufs[(gi % 2) * 2]
                wu = wi_bufs[(gi % 2) * 2 + 1]
                S.dma(wg[:, :, 0:ng * 128], V(dwi_r[:, :, g0 * 128:(g0 + ng) * 128], CONST), extra_reads=twi)
                S.dma(wu[:, :, 0:ng * 128], V(dwi_r[:, :, 2816 + g0 * 128:2816 + (g0 + ng) * 128], CONST),
                      extra_reads=twi)
                for j in range(ng):
                    fc = g0 + j
                    for part, wsrc in ((0, wg), (1, wu)):
                        pu = psb("B")
                        for k in range(8):
                            S.mm(pu, wsrc[:, k, j * 128:(j + 1) * 128], hTv(k, b * 512, (b + 1) * 512),
                                 start=(k == 0), stop=(k == 7))
                        if pend[0] is not None:
                            finish(*pend[0])
                        pend[0] = (pu, part, fc)
            finish(*pend[0])
            pend[0] = None
            for fc in range(22):
                wo = wo_bufs[fc % 4]
                S.dma(wo, V(dwo[fc * 128:(fc + 1) * 128, :], CONST), extra_reads=two)
                for j in range(4):
                    for half in range(2):
                        S.mm(PS[j * 2 + half], V(actT_ap[:, fc, j * 128:(j + 1) * 128], act_tiles[fc]),
                             wo[:, half * 512:(half + 1) * 512], start=(fc == 0), stop=(fc == 21))
            for j in range(4):
                tt = 4 * b + j
                xt = load_x_tile(out_d[s, tt * 128:(tt + 1) * 128, :], out_t[s][tt])
                for half in range(2):
                    S.tt(xt[:, half * 512:(half + 1) * 512], PS[j * 2 + half], xt[:, half * 512:(half + 1) * 512],
                         ALU.add)
                S.dma(V(out_d[s, tt * 128:(tt + 1) * 128, :], out_t[s][tt]), xt)
        arena_reset(newt)
        new = S.retile(wi_t, 32)
        mixT_t = [[new[c * 4 + b] for b in range(NQB)] for c in range(8)]

    if dbg:
        for c in range(8):
            for b in range(NQB):
                S.memset(V(mixT_ap[:, c, b * 512:(b + 1) * 512], mixT_t[c][b]), 0.0)
    for s in range(NS):
        rope_tables(s)
    for s in range(NS):
        cur_seq[0] = s
        for l in range(NL):
            P = layer_params(l)
            S.dma(gpp[:, 0:8], V(W["mix_norm_g"][l].rearrange("(k p) -> p k", p=128), CONST),
                  allow_slow_non_contiguous=True)
            S.dma(gpp[:, 8:16], V(W["ffn_norm_g"][l].rearrange("(k p) -> p k", p=128), CONST),
                  allow_slow_non_contiguous=True)
            g1, g2 = 0, 1
            if l == 0:
                src_ap = lambda tt, s=s: x_d[s, tt * 128:(tt + 1) * 128, :]
                src_tile = lambda tt: CONST
            else:
                src_ap = lambda tt, s=s: out_d[s, tt * 128:(tt + 1) * 128, :]
                src_tile = lambda tt, s=s: out_t[s][tt]
            for tt in range(NT):
                xt = load_x_tile(src_ap(tt), src_tile(tt))
                norm_tile_to_hT(xt, g1, tt)
            if dbg and s == 0 and l == 0:
                allh = S.retile(hT_t, 1)[0]
                dbgdump("hT", V(hT_ap, allh), [128, 8, SEQ])
                hT_t[:] = S.retile([allh], NQB)
            if "moba" in phases:
                moba(l, s, P)
            if "mla" in phases:
                mla(l, s, P)
            if "sb" in phases:
                sbmix(l, s, P)
            if "ssd" in phases:
                ssd(l, s, P)
            if dbg and s == 0 and l == 0:
                allmix = S.retile([t for row in mixT_t for t in row], 1)[0]
                dbgdump("mixT", V(mixT_ap, allmix), [128, 8, SEQ])
                new = S.retile([allmix], 32)
                mixT_t = [[new[c * 4 + b] for b in range(NQB)] for c in range(8)]
            if "ffn" in phases:
                out_proj_and_norm2(l, s, src_ap, src_tile, g2)
                ffn(l, s)
    S.emit()
    return nc, dbg_out


_CACHE = {}


def kernel(**inputs):
    n = 8
    NS = 2
    if "nc" not in _CACHE:
        _CACHE["nc"] = build(2, NS)[0]
    nc = _CACHE["nc"]
    hc = host_consts()
    x = np.ascontiguousarray(inputs["x"], dtype=np.float32)
    pos = np.ascontiguousarray(inputs["positions"], dtype=np.int32)
    in_maps = []
    for c in range(n):
        m = {"x": x[c * NS:(c + 1) * NS], "positions": pos[c * NS:(c + 1) * NS]}
        for w in WNAMES:
            m[w] = np.ascontiguousarray(inputs[w], dtype=np.float32)
        for k, a in hc.items():
            m["c_" + k] = a
        in_maps.append(m)
    res = run_bass_kernel_spmd(nc, in_maps, core_ids=list(range(n)))
    out = np.concatenate([np.asarray(r["out"], dtype=np.float32) for r in res.results], axis=0)
    return out
```

```python
import numpy as np
import ml_dtypes
from contextlib import ExitStack
import concourse.bass as bass
import concourse.mybir as mybir
from concourse.bass_utils import run_bass_kernel_spmd

F32 = mybir.dt.float32
BF16 = mybir.dt.bfloat16
I32 = mybir.dt.int32
AF = mybir.ActivationFunctionType
ALU = mybir.AluOpType
AX = mybir.AxisListType

ENGS = ("pe", "act", "dve", "pool", "sp")
NPOOL = 48
BIG = 32768.0
EPS = 1e-6
THETA = 10000.0


class Tile:
    __slots__ = ("name", "lw", "rd", "const")

    def __init__(self, name, const=False):
        self.name = name
        self.lw = []
        self.rd = {}
        self.const = const


class V:
    __slots__ = ("ap", "t")

    def __init__(self, ap, t):
        self.ap = ap
        self.t = t

    def __getitem__(self, k):
        return V(self.ap[k], self.t)


class Op:
    __slots__ = ("fn", "waits", "signal", "semval", "dma")

    def __init__(self, fn, waits, dma=None):
        self.fn = fn
        self.waits = waits
        self.signal = False
        self.semval = None
        self.dma = dma


class Sched:
    def __init__(self, nc):
        self.nc = nc
        self.ops = {e: [] for e in ENGS}
        self.waited = {e: {} for e in ENGS}
        self.pool_cnt = [0] * NPOOL
        self.pool_next = 0
        self.n = 0

    def tile(self, name=None, const=False):
        self.n += 1
        return Tile(name or f"t{self.n}", const)

    def retile(self, old, n_new):
        mw, mr = {}, {}
        for t in old:
            for d in t.lw:
                k = d[1] if d[0] == "e" else ("d", d[1])
                if k not in mw or mw[k][2] < d[2]:
                    mw[k] = d
            for k, d in t.rd.items():
                if k not in mr or mr[k][2] < d[2]:
                    mr[k] = d
        out = []
        for _ in range(n_new):
            t = self.tile()
            t.lw = list(mw.values())
            t.rd = dict(mr)
            out.append(t)
        return out

    def _need(self, eng, dep, waits):
        if dep is None:
            return
        if dep[0] == "e":
            _, x, idx = dep
            if x == eng and eng == "pe":
                return
            key, val = x, idx
        else:
            _, p, val = dep
            key = ("d", p)
        w = self.waited[eng]
        if w.get(key, -1) >= val:
            return
        w[key] = val
        waits.append(dep)
        if dep[0] == "e":
            self.ops[dep[1]][dep[2]].signal = True

    def _deps(self, eng, reads, writes):
        waits = []
        for v in reads:
            for d in v.t.lw:
                self._need(eng, d, waits)
        for v in writes:
            t = v.t
            for d in t.lw:
                self._need(eng, d, waits)
            for d in list(t.rd.values()):
                self._need(eng, d, waits)
        return waits

    def _mark(self, me, reads, writes):
        rkey = me[1] if me[0] == "e" else ("d", me[1])
        for v in reads:
            t = v.t
            if not t.const:
                t.rd[rkey] = me
        for v in writes:
            t = v.t
            t.lw = [me]
            t.rd = {}

    @staticmethod
    def _vs(xs):
        return [x for x in xs if isinstance(x, V)]

    def op(self, eng, fn, reads=(), writes=()):
        reads = self._vs(reads)
        writes = self._vs(writes)
        waits = self._deps(eng, reads, writes)
        idx = len(self.ops[eng])
        self.ops[eng].append(Op(fn, waits))
        self._mark(("e", eng, idx), reads, writes)

    def dma(self, out, in_, eng="sp", extra_reads=(), **kw):
        p = self.pool_next
        self.pool_next = (self.pool_next + 1) % NPOOL
        reads = [in_] + list(extra_reads)
        waits = self._deps(eng, reads, [out])
        if self.pool_cnt[p] > 0:
            self._need(eng, ("d", p, self.pool_cnt[p]), waits)
        self.pool_cnt[p] += 1
        val = self.pool_cnt[p]
        oap, iap = out.ap, in_.ap

        def fn(e, oap=oap, iap=iap, kw=kw):
            return e.dma_start(out=oap, in_=iap, **kw)
        self.ops[eng].append(Op(fn, waits, dma=(p, val)))
        self._mark(("d", p, val), reads, [out])

    def emit(self):
        nc = self.nc
        with ExitStack() as es:
            esem = {e: es.enter_context(nc.semaphore(f"s_{e}")) for e in ENGS}
            dsem = [es.enter_context(nc.semaphore(f"d_{i}")) for i in range(NPOOL)]
            for e in ENGS:
                c = 0
                for o in self.ops[e]:
                    if o.dma is None and o.signal:
                        c += 1
                        o.semval = c
            block = es.enter_context(nc.Block())
            engobj = {"pe": "tensor", "act": "scalar", "dve": "vector", "pool": "gpsimd", "sp": "sync"}

            def make(ename):
                def body(eng):
                    for o in self.ops[ename]:
                        for d in o.waits:
                            if d[0] == "e":
                                eng.wait_ge(esem[d[1]], self.ops[d[1]][d[2]].semval)
                            else:
                                eng.wait_ge(dsem[d[1]], 16 * d[2])
                        ins = o.fn(eng)
                        if o.dma is not None:
                            ins.then_inc(dsem[o.dma[0]], 16)
                        elif o.signal:
                            ins.then_inc(esem[ename], 1)
                    if ename == "sp":
                        for p in range(NPOOL):
                            if self.pool_cnt[p] > 0:
                                eng.wait_ge(dsem[p], 16 * self.pool_cnt[p])
                return body
            for ename in ENGS:
                getattr(block, engobj[ename])(make(ename))

    @staticmethod
    def _a(x):
        return x.ap if isinstance(x, V) else x

    def mm(self, out, lhsT, rhs, start=True, stop=True, **kw):
        a = self._a
        o, l, r = a(out), a(lhsT), a(rhs)
        self.op("pe", lambda e: e.matmul(o, l, r, start=start, stop=stop, **kw),
                reads=[lhsT, rhs], writes=[out])

    def transpose(self, out, in_, ident):
        a = self._a
        o, i, d = a(out), a(in_), a(ident)
        self.op("pe", lambda e: e.transpose(o, i, d), reads=[in_, ident], writes=[out])

    def act(self, out, in_, func, bias=None, scale=1.0, accum_out=None):
        a = self._a
        o, i = a(out), a(in_)
        kw = {"scale": a(scale)}
        if bias is not None:
            kw["bias"] = a(bias)
        if accum_out is not None:
            kw["accum_out"] = a(accum_out)
        self.op("act", lambda e: e.activation(o, i, func, **kw),
                reads=[in_, bias, scale], writes=[out, accum_out])

    def tt(self, out, in0, in1, op, eng="dve"):
        a = self._a
        o, x, y = a(out), a(in0), a(in1)
        self.op(eng, lambda e: e.tensor_tensor(o, x, y, op), reads=[in0, in1], writes=[out])

    def ts(self, out, in0, s1, op0, s2=None, op1=None, eng="dve"):
        a = self._a
        o, x, p, q = a(out), a(in0), a(s1), a(s2)
        kw = {}
        if op1 is not None:
            kw["op1"] = op1
        self.op(eng, lambda e: e.tensor_scalar(o, x, p, q, op0, **kw),
                reads=[in0, s1, s2], writes=[out])

    def stt(self, out, in0, scalar, in1, op0, op1, eng="dve"):
        a = self._a
        o, x, s, y = a(out), a(in0), a(scalar), a(in1)
        self.op(eng, lambda e: e.scalar_tensor_tensor(o, x, s, y, op0, op1),
                reads=[in0, scalar, in1], writes=[out])

    def copy(self, out, in_, eng="dve"):
        a = self._a
        o, i = a(out), a(in_)
        if eng == "act":
            self.op("act", lambda e: e.activation(o, i, AF.Copy), reads=[in_], writes=[out])
        else:
            self.op(eng, lambda e: e.tensor_copy(o, i), reads=[in_], writes=[out])

    def memset(self, out, val, eng="pool"):
        o = self._a(out)
        self.op(eng, lambda e: e.memset(o, val), reads=[], writes=[out])


def _bf(a):
    return np.asarray(a, dtype=np.float32).astype(ml_dtypes.bfloat16)


def host_consts():
    c = {}
    p = np.arange(128)
    c["ident"] = np.eye(128, dtype=np.float32)
    c["identb"] = _bf(np.eye(128))
    k = p[:, None]
    q = p[None, :]
    c["causneg"] = _bf(np.where(k <= q, 0.0, -BIG))
    c["strictneg"] = _bf(np.where(k < q, 0.0, -BIG))
    es = np.zeros((33, 32, 128), np.float32)
    for j in range(32):
        es[j, j, :] = BIG
    es[32, :, :] = -BIG
    c["esel"] = _bf(es)
    c["blk64"] = _bf((p[:, None] // 64 == p[None, :] // 64) / 64.0)
    c["on96"] = _bf(np.full((96, 96), 1.0 / 96))
    c["on192"] = _bf(np.full((128, 128), 1.0 / 192))
    c["on128"] = _bf(np.full((128, 128), 1.0 / 128))
    wn = np.full((65, 64), 1.0 / 64, np.float32)
    wn[64, :] = EPS
    c["wn"] = _bf(wn)
    c["negtri"] = _bf(np.where(p[:, None] >= p[None, :], -1.0, 0.0))
    c["onesb"] = _bf(np.ones((128, 128)))
    ra = np.zeros((128, 128), np.float32)
    for base in (0, 64):
        for d in range(32):
            ra[base + d + 32, base + d] = -1.0
            ra[base + d, base + d + 32] = 1.0
    c["rotA"] = _bf(ra)
    rm = np.zeros((96, 96), np.float32)
    for j in range(16):
        rm[80 + j, 64 + j] = -1.0
        rm[64 + j, 80 + j] = 1.0
    c["rotM"] = _bf(rm)
    c["triu"] = np.where(p[:, None] <= p[None, :], 1.0, 0.0).astype(np.float32)
    c["negmaskT"] = np.where(p[:, None] <= p[None, :], 0.0, -30000.0).astype(np.float32)
    pn = np.zeros((128, 8, 4, 8), np.float32)
    for cur in range(8):
        pn[:, cur, :, cur:] = -3.0e38
    c["pastneg"] = pn
    inv_a = (1.0 / (np.float32(THETA) ** (np.arange(0, 64, 2, dtype=np.float32) / np.float32(64)))).astype(np.float32)
    inv_m = (1.0 / (np.float32(THETA) ** (np.arange(0, 32, 2, dtype=np.float32) / np.float32(32)))).astype(np.float32)
    iv = np.zeros((128, 2), np.float32)
    iv[:, 0] = (inv_a[p % 32].astype(np.float64) / (2 * np.pi)).astype(np.float32)
    iv[64:96, 1] = (inv_m[(p[64:96] - 64) % 16].astype(np.float64) / (2 * np.pi)).astype(np.float32)
    c["invf"] = iv
    c["onesrow"] = _bf(np.ones((1, 128)))
    c["ones32"] = np.ones((128, 128), np.float32)
    return c


WNAMES = ["mix_norm_g", "w_in", "moba_qk_g", "mla_q_norm_g", "mla_kv_norm_g", "mla_w_uq", "mla_w_ukv",
          "mla_qk_g", "ssm_conv_w", "ssm_conv_b", "ssm_dt_bias", "ssm_a_log", "ssm_d", "ssm_norm_g",
          "head_out_g", "w_out", "ffn_norm_g", "ffn_w_in", "ffn_conv_w", "ffn_conv_b", "ffn_w_out"]
WSHAPES = {"mix_norm_g": (2, 1024), "w_in": (2, 1024, 2660), "moba_qk_g": (2, 2, 64), "mla_q_norm_g": (2, 192),
           "mla_kv_norm_g": (2, 128), "mla_w_uq": (2, 192, 384), "mla_w_ukv": (2, 128, 512), "mla_qk_g": (2, 2, 96),
           "ssm_conv_w": (2, 4, 512), "ssm_conv_b": (2, 512), "ssm_dt_bias": (2, 4), "ssm_a_log": (2, 4),
           "ssm_d": (2, 4), "ssm_norm_g": (2, 256), "head_out_g": (2, 3, 4, 64), "w_out": (2, 1024, 1024),
           "ffn_norm_g": (2, 1024), "ffn_w_in": (2, 1024, 5632), "ffn_conv_w": (2, 3, 5632),
           "ffn_conv_b": (2, 5632), "ffn_w_out": (2, 2816, 1024)}

SEQ = 2048
DM = 1024
NT = 16
NQB = 4


def build(NL=2, NS=2, dbg=False, phases=("moba", "mla", "sb", "ssd", "ffn")):
    nc = bass.Bass("TRN2", target_bir_lowering=False)
    S = Sched(nc)
    CONST = S.tile("const", const=True)

    def din(name, shape, dt=F32):
        return nc.dram_tensor(name, list(shape), dt, kind="ExternalInput").ap()

    x_d = din("x", [NS, SEQ, DM])
    pos_d = din("positions", [NS, SEQ], I32)
    W = {n: din(n, WSHAPES[n]) for n in WNAMES}
    hc = host_consts()
    CD = {n: din("c_" + n, a.shape, BF16 if a.dtype == ml_dtypes.bfloat16 else F32) for n, a in hc.items()}
    out_d = nc.dram_tensor("out", [NS, SEQ, DM], F32, kind="ExternalOutput").ap()
    out_t = [[S.tile(f"out{s}_{t}") for t in range(NT)] for s in range(NS)]
    dbg_out = {}

    def dbgdump(name, v, shape):
        if not dbg:
            return
        d = nc.dram_tensor("dbg_" + name, list(shape), v.ap.dtype, kind="ExternalOutput").ap()
        dbg_out[name] = d
        S.dma(V(d, S.tile()), v)

    cnt = [0]

    def sb(shape, dt=F32, name=None):
        cnt[0] += 1
        return nc.alloc_sbuf_tensor(name or f"sb{cnt[0]}", list(shape), dt).ap()

    def sbv(shape, dt=F32, name=None, const=False):
        return V(sb(shape, dt, name), S.tile(name, const=const))

    PS = [V(nc.alloc_psum_tensor(f"ps{i}", [128, 512], F32).ap(), S.tile(f"ps{i}")) for i in range(8)]
    ps_rr = {}

    def psb(role):
        banks = {"A": (0, 1), "B": (2, 3), "C": (4, 5), "D": (6, 7)}[role]
        i = ps_rr.get(role, 0)
        ps_rr[role] = i + 1
        return PS[banks[i % len(banks)]]

    C = {}
    for n, a in hc.items():
        v = sbv(a.shape, BF16 if a.dtype == ml_dtypes.bfloat16 else F32, name="k_" + n)
        S.dma(v, V(CD[n], CONST))
        v.t.const = False
        C[n] = v
    ident, identb = C["ident"], C["identb"]

    WB16 = {}

    def cast_weight(name, l, rows, cols):
        dst = nc.dram_tensor(f"{name}_b16_{l}", [rows, cols], BF16, kind="Internal").ap()
        src = W[name][l]
        tiles = []
        for r0 in range(0, rows, 512):
            for c0 in range(0, cols, 2048):
                r1, c1 = min(rows, r0 + 512), min(cols, c0 + 2048)
                t = S.tile()
                S.dma(V(dst[r0:r1, c0:c1], t), V(src[r0:r1, c0:c1], CONST), eng="pool")
                tiles.append(V(None, t))
        WB16[(name, l)] = (dst, tiles)

    for l in range(NL):
        cast_weight("w_in", l, 1024, 2660)
        cast_weight("mla_w_uq", l, 192, 384)
        cast_weight("mla_w_ukv", l, 128, 512)
        cast_weight("w_out", l, 1024, 1024)
        cast_weight("ffn_w_in", l, 1024, 5632)
        cast_weight("ffn_w_out", l, 2816, 1024)

    hT_ap = sb([128, 8, SEQ], BF16, "hT")
    hT_t = [S.tile(f"hT{b}") for b in range(NQB)]
    mixT_ap = sb([128, 8, SEQ], BF16, "mixT")
    mixT_t = [[S.tile(f"mx{c}_{b}") for b in range(NQB)] for c in range(8)]
    ARN = 21504
    arena_ap = sb([128, ARN], BF16, "arena")
    NQK = 32
    VOFF = NQK * 512
    arena_tiles = [S.tile(f"ar{i}") for i in range(NQK)] + [S.tile("arv")]

    def arena_reset(old):
        nonlocal arena_tiles
        arena_tiles = S.retile(old, NQK + 1)
    WBUF = sb([128, 8 * 1024], BF16, "wbuf")
    wbuf_t = [S.tile("wbuf")]
    rope_d = nc.dram_tensor("rope_scr", [NS * 4, 128, SEQ], BF16, kind="Internal").ap()
    rope_t = [[S.tile(f"rope{i}_{b}") for b in range(NQB)] for i in range(NS * 4)]
    selT = sbv([33, SEQ], BF16, "selT")
    xt_bufs = [sbv([128, DM], F32, f"xt{i}") for i in range(2)]
    gpp = sbv([128, 16], F32, "gpp")
    pp = sb([128, 64], F32, "pp")
    pp_t = S.tile("pp")
    small = {}

    rr = {}

    poolF = [sbv([128, 512], F32, f"poolF{i}") for i in range(7)]
    poolB = [sbv([128, 512], BF16, f"poolB{i}") for i in range(8)]

    def rot(key, shape, dt, n=2, ded=False):
        free = list(shape[1:])
        nel = int(np.prod(free))
        nbytes = nel * (2 if dt == BF16 else 4)
        pool = None
        if not ded and n >= 2:
            if dt in (F32, I32) and 256 < nbytes <= 2048:
                pool, pk = poolF, "poolF"
            elif dt == BF16 and 128 < nbytes <= 1024:
                pool, pk = poolB, "poolB"
        if pool is None:
            if key not in small:
                small[key] = [sbv(shape, dt) for _ in range(n)]
            i = rr.get(key, 0)
            rr[key] = i + 1
            return small[key][i % n]
        i = rr.get(pk, 0)
        rr[pk] = i + 1
        b = pool[i % len(pool)]
        ap = b.ap
        if dt == I32:
            ap = ap.bitcast(I32)
        ap = ap[0:shape[0], 0:nel]
        if len(free) == 2:
            ap = ap.rearrange("p (a b) -> p a b", a=free[0])
        return V(ap, b.t)

    def hTv(k, c0, c1):
        return V(hT_ap[:, k, c0:c1], hT_t[c0 // 512])

    def mixv(c, p0, p1, b):
        return V(mixT_ap[p0:p1, c, b * 512:(b + 1) * 512], mixT_t[c][b])

    def arv(off, n, parts=128, p0=0):
        assert off // 512 == (off + n - 1) // 512 and off + n <= VOFF, (off, n)
        return V(arena_ap[p0:p0 + parts, off:off + n], arena_tiles[off // 512])

    def arvv():
        return arena_tiles[NQK]

    def rstd_from(out, in_, scale, eps=EPS):
        S.act(out, in_, AF.Ln, bias=EPSC[eps][0:out.ap.shape[0]], scale=scale)
        S.act(out, out, AF.Exp, scale=-0.5)

    def silu_to(out, in_, bias=None):
        p0 = 0
        shp = list(in_.ap.shape)
        nel = int(np.prod(shp[1:]))
        vb = rot("siluv", [128, 512], F32, 1, ded=True)
        eb = rot("silue", [128, 512], F32, 1, ded=True)
        pbase = getattr(in_.ap, "base_partition", lambda: 0)()
        v = vb[pbase:pbase + shp[0], 0:nel]
        e = eb[pbase:pbase + shp[0], 0:nel]
        if len(shp) == 3:
            v = V(v.ap.rearrange("p (a b) -> p a b", a=shp[1]), v.t)
            e = V(e.ap.rearrange("p (a b) -> p a b", a=shp[1]), e.t)
        if bias is not None:
            S.ts(v, in_, bias, ALU.add)
        else:
            S.copy(v, in_)
        S.act(e, v, AF.Exp, scale=-1.0)
        S.ts(e, e, 1.0, ALU.add)
        S.op("dve", lambda en, o=e.ap: en.reciprocal(o, o), reads=[e], writes=[e])
        S.tt(out, v, e, ALU.mult)

    epsc = sbv([128, 2], F32, "epsc")
    S.memset(epsc[:, 0:1], EPS)
    S.memset(epsc[:, 1:2], 1.0)
    EPSC = {EPS: epsc[:, 0:1]}
    ONE_AP = epsc[:, 1:2]

    def load_w_cols(name, l, c0, ncols, rows=1024):
        nonlocal wbuf_t
        dst, tiles = WB16[(name, l)]
        k = rows // 128
        wbuf_t = S.retile(wbuf_t, 1)
        view = WBUF[:, 0:k * ncols].rearrange("p (k c) -> p k c", k=k)
        S.dma(V(view, wbuf_t[0]), V(dst.rearrange("(k p) c -> p k c", p=128)[:, :, c0:c0 + ncols], CONST),
              extra_reads=tiles)
        return V(view, wbuf_t[0])

    def layer_params(l):
        P = {}
        ppv = V(pp, pp_t)
        col = [0]

        def newcol(n=1):
            c0 = col[0]
            col[0] += n
            return c0

        def ld(dst_rows, c, src_ap):
            S.dma(V(pp[dst_rows[0]:dst_rows[1], c:c + 1], pp_t), V(src_ap, CONST))

        for i, nm in enumerate(("moba_gq", "moba_gk")):
            c = newcol()
            src = W["moba_qk_g"][l, i].rearrange("(d o) -> d o", o=1)
            ld((0, 64), c, src)
            ld((64, 128), c, src)
            P[nm] = c
        c = newcol()
        S.ts(ppv[:, c:c + 1], ppv[:, P["moba_gq"]:P["moba_gq"] + 1], 0.125, ALU.mult)
        P["moba_gq_s"] = c
        g = W["mla_q_norm_g"][l].rearrange("(d o) -> d o", o=1)
        c = newcol(); ld((0, 128), c, g[0:128]); P["mla_gqA"] = c
        c = newcol(); ld((0, 64), c, g[128:192]); P["mla_gqB"] = c
        c = newcol(); ld((0, 128), c, W["mla_kv_norm_g"][l].rearrange("(d o) -> d o", o=1)); P["mla_gkv"] = c
        c = newcol(); ld((0, 96), c, W["mla_qk_g"][l, 0].rearrange("(d o) -> d o", o=1)); P["mla_qg"] = c
        c2 = newcol()
        S.ts(ppv[0:96, c2:c2 + 1], ppv[0:96, c:c + 1], float(96 ** -0.5), ALU.mult)
        P["mla_qg_s"] = c2
        c = newcol(); ld((0, 96), c, W["mla_qk_g"][l, 1].rearrange("(d o) -> d o", o=1)); P["mla_kg"] = c
        c = newcol(12)
        S.dma(V(pp[0:64, c:c + 12], pp_t), V(W["head_out_g"][l].rearrange("m h d -> d (m h)"), CONST),
              allow_slow_non_contiguous=True)
        P["hog"] = c
        c = newcol(16)
        for tap in range(4):
            S.dma(V(pp[:, c + tap * 4:c + tap * 4 + 4], pp_t),
                  V(W["ssm_conv_w"][l, tap].rearrange("(c p) -> p c", p=128), CONST), allow_slow_non_contiguous=True)
        P["cw"] = c
        c = newcol(4)
        S.dma(V(pp[:, c:c + 4], pp_t), V(W["ssm_conv_b"][l].rearrange("(c p) -> p c", p=128), CONST),
              allow_slow_non_contiguous=True)
        P["cb"] = c
        for nm, key in (("dtb", "ssm_dt_bias"), ("alog", "ssm_a_log"), ("dsk", "ssm_d")):
            c = newcol(4)
            S.dma(V(pp[:, c:c + 4], pp_t),
                  V(W[key][l].rearrange("(o h) -> o h", o=1).to_broadcast([128, 4]), CONST))
            P[nm] = c
        c = newcol(4)
        S.act(ppv[:, c:c + 4], ppv[:, P["alog"]:P["alog"] + 4], AF.Exp)
        S.ts(ppv[:, c:c + 4], ppv[:, c:c + 4], -1.0, ALU.mult)
        P["ahead"] = c
        assert col[0] <= 64
        return P

    def rope_tables(s):
        for b in range(NQB):
            posi = rot("posi", [128, 512], I32)
            S.dma(posi, V(pos_d[s:s + 1, b * 512:(b + 1) * 512].to_broadcast([128, 512]), CONST))
            posf = rot("posf", [128, 512], F32, 1, ded=True)
            S.copy(posf, posi)
            for ti, (col, phase) in enumerate(((0, 0.25), (0, 0.0), (1, 0.25), (1, 0.0))):
                ang = rot("ang", [128, 512], F32)
                S.ts(ang, posf, C["invf"][:, col:col + 1], ALU.mult, float(phase), ALU.add)
                ai = rot("angi", [128, 512], I32)
                S.copy(ai, ang)
                af = rot("angf", [128, 512], F32)
                S.copy(af, ai)
                fr = rot("frac", [128, 512], F32)
                S.tt(fr, ang, af, ALU.subtract)
                m1 = rot("frm", [128, 512], F32)
                S.ts(m1, fr, 0.5, ALU.is_gt)
                S.tt(fr, fr, m1, ALU.subtract)
                m2 = rot("frm", [128, 512], F32)
                S.ts(m2, fr, -0.5, ALU.is_lt)
                S.tt(fr, fr, m2, ALU.add)
                S.ts(fr, fr, 0.4999999, ALU.min, -0.4999999, ALU.max)
                tab = rot("tab", [128, 512], BF16)
                S.act(tab, fr, AF.Sin, scale=float(2 * np.pi))
                S.dma(V(rope_d[s * 4 + ti, :, b * 512:(b + 1) * 512], rope_t[s * 4 + ti][b]), tab)

    cur_seq = [0]

    def rope_blk(ti, b, parts=128):
        v = rot("ropeb%d" % (ti % 2), [128, 512], BF16, 2, ded=True)
        tj = cur_seq[0] * 4 + ti
        S.dma(v[0:parts], V(rope_d[tj, 0:parts, b * 512:(b + 1) * 512], rope_t[tj][b]))
        return v[0:parts]

    def norm_tile_to_hT(xt, gi, tt):
        ss2 = rot("ss", [128, 2], F32, 4)
        for half in range(2):
            S.act(rot("jk", [128, 512], BF16), xt[:, half * 512:(half + 1) * 512], AF.Square,
                  accum_out=ss2[:, half:half + 1])
        ss = rot("ssum", [128, 1], F32, 4)
        S.tt(ss, ss2[:, 0:1], ss2[:, 1:2], ALU.add)
        rs = rot("rs", [128, 1], F32, 4)
        rstd_from(rs, ss, 1.0 / DM)
        for half in range(2):
            hn = rot("hn", [128, 512], F32)
            S.ts(hn, xt[:, half * 512:(half + 1) * 512], rs, ALU.mult)
            pb = psb("A")
            for j in range(4):
                S.transpose(pb[:, j * 128:(j + 1) * 128], hn[:, j * 128:(j + 1) * 128], ident)
            dst = V(hT_ap[:, half * 4:(half + 1) * 4, tt * 128:(tt + 1) * 128], hT_t[tt // 4])
            src = V(pb.ap.rearrange("p (j t) -> p j t", j=4), pb.t)
            c0 = gi * 8 + half * 4
            S.tt(dst, src, V(gpp.ap[:, c0:c0 + 4].unsqueeze(2).to_broadcast([128, 4, 128]), gpp.t), ALU.mult)

    def load_x_tile(src_ap, tile_obj):
        xt = xt_bufs[rr.get("xt", 0) % 2]
        rr["xt"] = rr.get("xt", 0) + 1
        S.dma(xt, V(src_ap, tile_obj))
        return xt

    def proj_fm(wv, c0, M, b, out_ps):
        for k in range(8):
            S.mm(out_ps[0:M, :], wv[:, k, c0:c0 + M], hTv(k, b * 512, (b + 1) * 512), start=(k == 0), stop=(k == 7))

    def proj_tm(wv, c0, N, tt, out_ps_cols):
        for k in range(8):
            S.mm(out_ps_cols, hTv(k, tt * 128, (tt + 1) * 128), wv[:, k, c0:c0 + N], start=(k == 0), stop=(k == 7))

    def head_norm_store(po, has_r, hog_col, c, h, b):
        sq = rot("hsq", [65, 512], BF16)
        nr = 65 if has_r else 64
        S.act(sq[0:nr], po[0:nr], AF.Square)
        pm = psb("C")
        S.mm(pm[0:64, :], C["wn"][0:nr, :], sq[0:nr])
        rs = rot("hrs", [64, 512], F32)
        if has_r:
            S.act(rs, pm[0:64, :], AF.Ln)
        else:
            S.act(rs, pm[0:64, :], AF.Ln, bias=EPSC[EPS][0:64])
        S.act(rs, rs, AF.Exp, scale=-0.5)
        p0 = (h % 2) * 64
        S.stt(mixv(c, p0, p0 + 64, b), po[0:64, :], V(pp[0:64, hog_col:hog_col + 1], pp_t), rs, ALU.mult, ALU.mult)

    vaug_ap = arena_ap[:, VOFF:VOFF + 16 * 4 * 66].rearrange("p (t h d) -> p t h d", t=16, h=4)

    def vaug(tt, h, n=65):
        return V(vaug_ap[:, tt, h, 0:n], arvv())

    def softmax_attention(kfn, qfn, mask_fn, hog_col, cbase):
        for h in range(4):
            c = h // 2
            for b in range(NQB):
                po = psb("D")
                nk = 4 * b + 4
                pend = None
                for i in range(nk):
                    r = i - 4 * b
                    psx = psb("A")
                    q0, extra = mask_fn(psx, h, b, i, r)
                    S.mm(psx[:, q0:512], kfn(h, i), qfn(h, b, q0), start=True, stop=(len(extra) == 0))
                    for ei, (eo, el, er) in enumerate(extra):
                        S.mm(eo, el, er, start=False, stop=(ei == len(extra) - 1))
                    pT = rot("pT", [128, 512], BF16, 3)
                    S.act(pT[:, q0:512], psx[:, q0:512], AF.Exp)
                    if pend is not None:
                        S.mm(*pend[0], **pend[1])
                    pend = ((po[0:65, q0:512], vaug(i, h), pT[:, q0:512]), dict(start=(i == 0), stop=(i == nk - 1)))
                S.mm(*pend[0], **pend[1])
                head_norm_store(po, True, hog_col + h, cbase + c, h, b)

    kmT = sbv([128, 4, 8], BF16, "kmT")

    def moba(l, s, P):
        wv = load_w_cols("w_in", l, 0, 768)
        qT = lambda c, b: arv(c * 2048 + b * 512, 512)
        kpad = lambda h, c0, n: arv(4096 + h * 2048 + c0, n)
        for i in range(8, 24):
            S.memset(V(arena_ap[:, i * 512:(i + 1) * 512], arena_tiles[i]), 0.0)
        S.memset(V(arena_ap[:, VOFF:VOFF + 16 * 4 * 66], arvv()), 1.0)
        S.memset(kmT, 0.0)
        for b in range(NQB):
            cosb = rope_blk(0, b)
            sinb = rope_blk(1, b)
            for which in range(2):
                gcol = P["moba_gq_s"] if which == 0 else P["moba_gk"]
                for c in range(2):
                    pq = psb("B")
                    proj_fm(wv, which * 256 + c * 128, 128, b, pq)
                    sq = rot("msq", [128, 512], BF16)
                    S.act(sq, pq, AF.Square)
                    pm = psb("C")
                    S.mm(pm, C["blk64"], sq)
                    rs = rot("mrs", [128, 512], F32)
                    rstd_from(rs, pm, 1.0)
                    qn = rot("mqn", [128, 512], BF16)
                    S.stt(qn, pq, V(pp[:, gcol:gcol + 1], pp_t), rs, ALU.mult, ALU.mult)
                    pr = psb("C")
                    S.mm(pr, C["rotA"], qn)
                    t1 = rot("mt1", [128, 512], F32)
                    S.tt(t1, qn, cosb, ALU.mult, eng="pool")
                    t2 = rot("mt2", [128, 512], F32)
                    S.tt(t2, pr, sinb, ALU.mult)
                    if which == 0:
                        S.tt(qT(c, b), t1, t2, ALU.add)
                    else:
                        ksum = rot("mks", [128, 512], F32)
                        S.tt(ksum, t1, t2, ALU.add)
                        for hh in range(2):
                            h = 2 * c + hh
                            S.copy(kpad(h, b * 512, 512)[hh * 64:(hh + 1) * 64], ksum[hh * 64:(hh + 1) * 64],
                                   eng=("act" if hh else "pool"))
                        km = rot("mkm", [128, 2], F32)
                        S.op("dve", lambda e, o=km.ap, i=ksum.ap: e.reduce_sum(
                            o, i.rearrange("p (n j) -> p n j", n=2), AX.X), reads=[ksum], writes=[km])
                        for hh in range(2):
                            h = 2 * c + hh
                            S.ts(kmT[hh * 64:(hh + 1) * 64, h, 2 * b:2 * b + 2], km[hh * 64:(hh + 1) * 64],
                                 1.0 / 256, ALU.mult)
        for tt in range(NT):
            pv = psb("B")
            proj_tm(wv, 512, 256, tt, pv[:, 0:256])
            S.copy(V(vaug_ap[:, tt, :, 0:64], arvv()),
                   V(pv.ap[:, 0:256].rearrange("p (h d) -> p h d", h=4), pv.t), eng="act")
        S.memset(selT[32:33, :], 1.0)
        for qt in range(NT):
            pg = psb("C")
            for h in range(4):
                c = h // 2
                S.mm(pg[:, h * 8:(h + 1) * 8], qT(c, qt // 4)[:, (qt % 4) * 128:(qt % 4 + 1) * 128], kmT[:, h, :])
            gm = rot("gm", [128, 32], F32)
            S.tt(gm, pg[:, 0:32], V(C["pastneg"].ap[:, qt // 2].rearrange("p h n -> p (h n)"), C["pastneg"].t), ALU.add)
            m8 = rot("m8", [128, 4, 8], F32)
            for h in range(4):
                S.op("dve", lambda e, o=m8.ap[:, h, :], i=gm.ap[:, h * 8:(h + 1) * 8]: e.max(o, i),
                     reads=[gm], writes=[m8])
            thr = rot("thr", [128, 4], F32)
            S.ts(thr, V(m8.ap[:, :, 2], m8.t), -1.0e30, ALU.max)
            sel = rot("sel", [128, 32], F32)
            S.tt(V(sel.ap.rearrange("p (h n) -> p h n", h=4), sel.t), V(gm.ap.rearrange("p (h n) -> p h n", h=4), gm.t),
                 V(thr.ap.unsqueeze(2).to_broadcast([128, 4, 8]), thr.t), ALU.is_ge)
            pt = psb("C")
            S.transpose(pt[0:32, 0:128], sel, ident)
            S.copy(selT[0:32, qt * 128:(qt + 1) * 128], pt[0:32, 0:128], eng="act")

        if dbg and s == 0 and l == 0:
            allar = S.retile(arena_tiles, 1)[0]
            dbgdump("moba_arena", V(arena_ap, allar), [128, ARN])
            arena_reset([allar])
            dbgdump("moba_selT", selT, [33, SEQ])
            dbgdump("moba_kmT", kmT, [128, 4, 8])

        def mask_fn(psx, h, b, i, r):
            q0 = 0 if r < 0 else (128 * r if r < 2 else max(256, 128 * r))
            n = i // 2
            if r < 0:
                return q0, [(psx, C["esel"][:, h * 8 + n, :], selT[:, b * 512:(b + 1) * 512])]
            ex = []
            if r < 2:
                ex.append((psx[:, 256:512], C["esel"][:, h * 8 + n, :], selT[:, b * 512 + 256:(b + 1) * 512]))
            ex.append((psx[:, 128 * r:128 * (r + 1)], identb, C["causneg"]))
            return q0, ex

        softmax_attention(lambda h, i: kpad(h, i * 128, 128), lambda h, b, q0: qT(h // 2, b)[:, q0:512],
                          mask_fn, P["hog"] + 0, 0)

    wuq = sbv([128, 2, 384], BF16, "wuq")
    wkv = sbv([128, 512], BF16, "wkv")

    def mla(l, s, P):
        wv = load_w_cols("w_in", l, 768, 352)
        dq, tq = WB16[("mla_w_uq", l)]
        S.dma(wuq[:, 0, :], V(dq[0:128, :], CONST), extra_reads=tq)
        S.dma(wuq[0:64, 1, :], V(dq[128:192, :], CONST), extra_reads=tq)
        dk, tk = WB16[("mla_w_ukv", l)]
        S.dma(wkv, V(dk, CONST), extra_reads=tk)
        qf = lambda h, c0, n: arv(h * 2048 + c0, n, parts=96)
        kf = lambda h, c0, n: arv(8192 + h * 2048 + c0, n, parts=96)
        S.memset(V(arena_ap[:, VOFF:VOFF + 16 * 4 * 66], arvv()), 1.0)
        ppc = lambda col, p0, p1: V(pp[p0:p1, col:col + 1], pp_t)

        def qk_finish(pre, gcol, dstv, cosb, sinb):
            sq = rot("lsq", [96, 512], BF16)
            S.act(sq, pre, AF.Square)
            pm = psb("C")
            S.mm(pm[0:96, :], C["on96"], sq)
            rs = rot("lrs", [96, 512], F32)
            rstd_from(rs, pm[0:96, :], 1.0)
            qn = rot("lqn", [96, 512], BF16)
            S.stt(qn, pre, ppc(gcol, 0, 96), rs, ALU.mult, ALU.mult)
            pr = psb("C")
            S.mm(pr[0:96, :], C["rotM"], qn)
            t1 = rot("lt1", [96, 512], F32)
            S.tt(t1, qn, cosb, ALU.mult, eng="pool")
            t2 = rot("lt2", [96, 512], F32)
            S.tt(t2, pr[0:96, :], sinb, ALU.mult)
            S.tt(dstv, t1, t2, ALU.add)

        for b in range(NQB):
            cosb = rope_blk(2, b, 96)
            sinb = rope_blk(3, b, 96)
            pA = psb("B"); proj_fm(wv, 0, 128, b, pA)
            pB = psb("B"); proj_fm(wv, 128, 64, b, pB)
            sqA = rot("lsqA", [128, 512], BF16)
            sqB = rot("lsqB", [64, 512], BF16)
            S.act(sqA, pA, AF.Square)
            S.act(sqB, pB[0:64, :], AF.Square)
            pm = psb("C")
            S.mm(pm, C["on192"], sqA, start=True, stop=False)
            S.mm(pm, C["on192"][0:64, :], sqB, start=False, stop=True)
            rs = rot("lrsq", [128, 512], F32)
            rstd_from(rs, pm, 1.0)
            cqA = rot("cqA", [128, 512], BF16, 1)
            cqB = rot("cqB", [64, 512], BF16, 1)
            S.stt(cqA, pA, ppc(P["mla_gqA"], 0, 128), rs, ALU.mult, ALU.mult)
            S.stt(cqB, pB[0:64, :], ppc(P["mla_gqB"], 0, 64), rs[0:64], ALU.mult, ALU.mult)
            pC = psb("B"); proj_fm(wv, 192, 128, b, pC)
            sqC = rot("lsqA", [128, 512], BF16)
            S.act(sqC, pC, AF.Square)
            pm2 = psb("C")
            S.mm(pm2, C["on128"], sqC)
            rs2 = rot("lrsq", [128, 512], F32)
            rstd_from(rs2, pm2, 1.0)
            ckv = rot("ckv", [128, 512], BF16, 1)
            S.stt(ckv, pC, ppc(P["mla_gkv"], 0, 128), rs2, ALU.mult, ALU.mult)
            pD = psb("B"); proj_fm(wv, 320, 32, b, pD)
            kpre = rot("kpre", [96, 512], F32, 1)
            S.copy(kpre[64:96, :], pD[0:32, :], eng="act")
            for h in range(4):
                pq = psb("B")
                S.mm(pq[0:96, :], wuq[:, 0, h * 96:(h + 1) * 96], cqA, start=True, stop=False)
                S.mm(pq[0:96, :], wuq[0:64, 1, h * 96:(h + 1) * 96], cqB, start=False, stop=True)
                qk_finish(pq[0:96, :], P["mla_qg_s"], qf(h, b * 512, 512), cosb, sinb)
                pk = psb("B")
                S.mm(pk[0:64, :], wkv[:, h * 128:h * 128 + 64], ckv)
                S.copy(kpre[0:64, :], pk[0:64, :], eng="act")
                qk_finish(kpre, P["mla_kg"], kf(h, b * 512, 512), cosb, sinb)
            for j in range(4):
                tt = 4 * b + j
                pv = psb("B")
                S.mm(pv[:, 0:256], ckv[:, j * 128:(j + 1) * 128],
                     V(wkv.ap.rearrange("p (h t d) -> p h t d", h=4, t=2)[:, :, 1, :], wkv.t))
                S.copy(V(vaug_ap[:, tt, :, 0:64], arvv()),
                       V(pv.ap[:, 0:256].rearrange("p (h d) -> p h d", h=4), pv.t), eng="act")

        def mask_fn(psx, h, b, i, r):
            q0 = 0 if r < 0 else 128 * r
            if r < 0:
                return q0, []
            return q0, [(psx[:, q0:q0 + 128], identb, C["causneg"])]

        if dbg and s == 0 and l == 0:
            allar = S.retile(arena_tiles, 1)[0]
            dbgdump("mla_arena", V(arena_ap, allar), [128, ARN])
            arena_reset([allar])
        softmax_attention(lambda h, i: kf(h, i * 128, 128), lambda h, b, q0: qf(h, b * 512, 512)[:, q0:512],
                          mask_fn, P["hog"] + 4, 2)

    def sbmix(l, s, P):
        wv = load_w_cols("w_in", l, 1120, 768)
        qT = lambda c, b: arv(c * 2048 + b * 512, 512)
        kpad = lambda h, c0, n: arv(4096 + h * 2048 + c0, n)
        vs_ap = arena_ap[:, VOFF:VOFF + 16 * 256].rearrange("p (t h d) -> p t h d", t=16, h=4)
        vsv = lambda tt, h: V(vs_ap[:, tt, h, :], arvv())
        for i in range(8, 24):
            S.memset(V(arena_ap[:, i * 512:(i + 1) * 512], arena_tiles[i]), 0.0)
        for c in range(2):
            for b in range(NQB):
                pq = psb("B")
                proj_fm(wv, c * 128, 128, b, pq)
                S.act(qT(c, b), pq, AF.Copy, scale=0.125)
                pk = psb("B")
                proj_fm(wv, 256 + c * 128, 128, b, pk)
                for hh in range(2):
                    h = 2 * c + hh
                    S.copy(kpad(h, b * 512, 512)[hh * 64:(hh + 1) * 64], pk[hh * 64:(hh + 1) * 64, :],
                           eng=("dve" if hh == 0 else "act"))
        for tt in range(NT):
            pv = psb("B")
            proj_tm(wv, 512, 256, tt, pv[:, 0:256])
            S.copy(V(vs_ap[:, tt, :, :], arvv()), V(pv.ap[:, 0:256].rearrange("p (h d) -> p h d", h=4), pv.t),
                   eng="act")
        def sb_step(st, b, i, nk):
            h, c, po, Rs = st["h"], st["c"], st["po"], st["Rs"]
            first = st["first"]
            r = i - 4 * b
            q0 = 0 if r < 0 else 128 * r
            w = slice(q0, 512)
            psx = psb("A")
            S.mm(psx[:, w], kpad(h, i * 128, 128), qT(c, b)[:, w], start=True, stop=False)
            if r >= 0:
                S.mm(psx[:, q0:q0 + 128], identb, C["strictneg"], start=False, stop=False)
            e = rot("sbe", [128, 512], F32, 2)
            S.act(e[:, w], psx[:, w], AF.Exp)
            sp = rot("sbsp", [128, 512], BF16, 2)
            S.act(sp[:, w], e[:, w], AF.Ln, bias=ONE_AP)
            S.mm(psx[:, w], C["negtri"], sp[:, w], start=False, stop=True, skip_group_check=True)
            a = rot("sba", [128, 512], BF16, 3)
            if first:
                S.act(a[:, w], psx[:, w], AF.Exp)
            else:
                d = rot("sbd", [128, 512], F32, 2)
                S.tt(d[:, w], psx[:, w], Rs[:, w], ALU.subtract)
                S.act(a[:, w], d[:, w], AF.Exp)
            if i > 0:
                pt = psb("C")
                S.mm(pt[:, w], C["onesb"], sp[:, w])
                if first:
                    if q0 > 0:
                        S.memset(Rs[:, 0:q0], 0.0, eng="pool")
                    S.copy(Rs[:, w], pt[:, w])
                else:
                    S.tt(Rs[:, w], Rs[:, w], pt[:, w], ALU.add)
            S.mm(po[0:64, w], vsv(i, h), a[:, w], start=(i == nk - 1), stop=(i == 0),
                 skip_group_check=True)
            st["first"] = False

        for hp in (0, 2):
            for b in range(NQB):
                nk = 4 * b + 4
                sts = [dict(h=h, c=h // 2, po=psb("D"), Rs=rot("sbR", [128, 512], F32, 2, ded=True), first=True)
                       for h in (hp, hp + 1)]
                for i in range(nk - 1, -1, -1):
                    for st in sts:
                        sb_step(st, b, i, nk)
                for st in sts:
                    head_norm_store(st["po"], False, P["hog"] + 8 + st["h"], 4 + st["c"], st["h"], b)

    diagw = sbv([128, 16, 128], BF16, "diagw")
    diagD = sbv([128, 4, 128], BF16, "diagD")

    def ssd(l, s, P):
        wv = load_w_cols("w_in", l, 1888, 772)
        ppc = lambda col, n=1: V(pp[:, col:col + n], pp_t)
        XW = 2056
        ssd_t = S.retile(arena_tiles, 3)
        xbc = lambda cc, c0, n: V(arena_ap[:, cc * XW + c0: cc * XW + c0 + n], ssd_t[0])
        BT0 = 4 * XW
        btpad = lambda g, c0, n: V(arena_ap[:, BT0 + g * 2048 + c0: BT0 + g * 2048 + c0 + n], ssd_t[1])
        CT0 = BT0 + 4096
        ctpad = lambda g, c0, n: V(arena_ap[:, CT0 + g * 2048 + c0: CT0 + g * 2048 + c0 + n], ssd_t[2])
        assert CT0 + 4096 <= ARN
        S.memset(V(arena_ap[:, 0:BT0], ssd_t[0]), 0.0)
        S.memset(V(arena_ap[:, BT0:CT0], ssd_t[1]), 0.0)
        S.memset(V(arena_ap[:, CT0:CT0 + 4096], ssd_t[2]), 0.0)
        for tap in range(4):
            for cc in range(4):
                S.ts(diagw[:, tap * 4 + cc, :], identb, ppc(P["cw"] + tap * 4 + cc), ALU.mult, eng="pool")
        for h in range(4):
            S.ts(diagD[:, h, :], identb, ppc(P["dsk"] + h), ALU.mult, eng="pool")
        gn = rot("ssm_gn", [128, 256], F32, 1)
        S.dma(gn, V(W["ssm_norm_g"][l].rearrange("(o c) -> o c", o=1).to_broadcast([128, 256]), CONST))
        for cc in range(4):
            for b in range(NQB):
                px = psb("B")
                proj_fm(wv, 256 + cc * 128, 128, b, px)
                S.copy(xbc(cc, 3 + b * 512, 512), px, eng=("act" if (cc + b) % 2 else "dve"))
        for cc in (2, 3):
            for b in range(NQB):
                pc = psb("B")
                for tap in range(4):
                    S.mm(pc, diagw[:, tap * 4 + cc, :], xbc(cc, b * 512 + tap, 512), start=(tap == 0), stop=(tap == 3))
                for g in range(2):
                    dst = (btpad if cc == 2 else ctpad)(g, b * 512, 512)
                    silu_to(dst[g * 64:(g + 1) * 64], pc[g * 64:(g + 1) * 64, :],
                            bias=V(pp[g * 64:(g + 1) * 64, P["cb"] + cc:P["cb"] + cc + 1], pp_t))
        import os
        stop = int(os.environ.get("SSD_STOP", "99"))
        if stop <= 1:
            arena_reset(ssd_t); return
        hst = rot("hst", [128, 4, 64], F32, 1)
        S.memset(hst, 0.0)
        Bp = rot("Bp", [128, 2, 128], BF16, 1)
        S.memset(Bp, 0.0)
        for tt in range(NT):
            t0 = tt * 128
            pz = psb("B")
            proj_tm(wv, 0, 256, tt, pz[:, 0:256])
            if not os.environ.get("SSD_NODT"):
                proj_tm(wv, 768, 4, tt, pz[:, 256:260])
            else:
                proj_tm(wv, 760, 12, tt, pz[:, 248:260])
            sub = int(os.environ.get("SSD_SUB", "99"))
            if sub <= 0:
                continue
            zs = rot("zs", [128, 256], F32, 1, ded=True)
            silu_to(zs, pz[:, 0:256])
            if sub <= 1:
                continue
            dtv = rot("dtv", [128, 4], F32)
            S.tt(dtv, pz[:, 256:260], ppc(P["dtb"], 4), ALU.add)
            S.act(dtv, dtv, AF.Exp)
            S.act(dtv, dtv, AF.Ln, bias=ONE_AP)
            av = rot("av", [128, 4], F32)
            S.tt(av, dtv, ppc(P["ahead"], 4), ALU.mult)
            if sub <= 2:
                continue
            if sub == 48:
                xs = rot("xs", [128, 256], BF16)
                S.memset(xs, 0.5)
            pcv = psb("C")
            for cc in range(3 if sub != 48 else 0):
                for tap in range(4):
                    S.mm(pcv[:, cc * 128:(cc + 1) * 128], diagw[:, tap * 4 + cc, :],
                         xbc(cc, t0 + (tap if sub != 43 else 2 * (tap // 2)), 128),
                         start=(tap == 0), stop=(tap == 3))
            cvs = rot("cvs", [128, 3, 128], BF16)
            for cc in range(3 if sub != 48 else 0):
                silu_to(cvs[:, cc, :], pcv[:, cc * 128:(cc + 1) * 128], bias=ppc(P["cb"] + cc))
            if sub == 50:
                xtr = rot("xtr", [128, 3, 128], BF16)
                for cc in range(3):
                    S.dma(xtr[:, cc, :], cvs[:, cc, :], transpose=True)
                ptr = psb("C")
                S.mm(ptr[:, 0:384], identb, V(xtr.ap.rearrange("p a b -> p (a b)"), xtr.t))
                continue
            if sub == 49:
                pdum = psb("C")
                for cc in range(3):
                    S.mm(pdum[:, cc * 128:(cc + 1) * 128], identb, C["causneg"])
                continue
            if sub <= 3:
                continue
            if sub == 40:
                xtmp = rot("xtmp", [128, 3, 128], BF16)
                S.copy(xtmp, cvs)
                continue
            if sub == 41 or sub == 43 or sub == 47:
                ptr = psb("C")
                S.mm(ptr[:, 0:128], cvs[:, 0, :], identb)
                continue
            if sub == 42:
                ptr = psb("A")
                for cc in range(3):
                    S.mm(ptr[:, cc * 128:(cc + 1) * 128], cvs[:, cc, :], identb)
                continue
            if sub != 48:
                ptr = psb("C")
                for cc in range(3):
                    S.mm(ptr[:, cc * 128:(cc + 1) * 128], cvs[:, cc, :], identb)
                if sub <= 4:
                    continue
                xs = rot("xs", [128, 256], BF16)
                S.copy(xs, ptr[:, 0:256], eng="act")
                for g in range(2):
                    S.copy(Bp[:, g, g * 64:(g + 1) * 64], ptr[:, 256 + g * 64:256 + (g + 1) * 64],
                           eng=("dve" if g else "act"))
            if stop <= 2:
                continue
            abc = rot("abc", [128, 4, 128], F32)
            for h in range(4):
                S.ts(abc[:, h, :], C["ones32"], av[:, h:h + 1], ALU.mult, eng="pool")
            pac = psb("C")
            for h in range(4):
                S.mm(pac[:, h * 128:(h + 1) * 128], abc[:, h, :], C["triu"])
            pcol = psb("B")
            S.mm(pcol[:, 0:4], C["triu"], av)
            acs = rot("acs", [128, 4], F32)
            S.copy(acs, pcol[:, 0:4])
            alast = rot("alast", [128, 4], F32)
            S.copy(alast, V(pac.ap.rearrange("p (h l) -> p h l", h=4)[:, :, 127], pac.t))
            segm = rot("segm", [128, 4, 128], F32)
            for h in range(4):
                S.stt(segm[:, h, :], pac[:, h * 128:(h + 1) * 128], acs[:, h:h + 1], C["negmaskT"], ALU.subtract, ALU.add)
            LT = rot("LT", [128, 4, 128], F32)
            S.act(LT, segm, AF.Exp)
            EA = rot("EA", [128, 4, 128], F32)
            S.act(EA, V(pac.ap.rearrange("p (h l) -> p h l", h=4), pac.t), AF.Exp)
            if stop <= 3:
                continue
            pG = psb("C")
            for g in range(2):
                S.mm(pG[:, g * 128:(g + 1) * 128], btpad(g, t0, 128), ctpad(g, t0, 128))
            MT = rot("MT", [128, 4, 128], BF16)
            CsT = rot("CsT", [128, 4, 128], BF16)
            for h in range(4):
                g = h // 2
                S.tt(MT[:, h, :], pG[:, g * 128:(g + 1) * 128], LT[:, h, :], ALU.mult)
                S.tt(CsT[:, h, :], ctpad(g, t0, 128), EA[:, h, :], ALU.mult, eng="pool")
            xdt = rot("xdt", [128, 4, 64], BF16)
            S.tt(xdt, V(xs.ap.rearrange("p (h d) -> p h d", h=4), xs.t),
                 V(dtv.ap.unsqueeze(2).to_broadcast([128, 4, 64]), dtv.t), ALU.mult)
            dec = rot("dec", [128, 4], F32)
            S.tt(dec, alast, acs, ALU.subtract)
            S.act(dec, dec, AF.Exp)
            xdd = rot("xdd", [128, 4, 64], BF16)
            S.tt(xdd, xdt, V(dec.ap.unsqueeze(2).to_broadcast([128, 4, 64]), dec.t), ALU.mult)
            cd = rot("cd", [128, 4], F32)
            S.act(cd, alast, AF.Exp)
            prevb = rot("prevb", [128, 4, 64], BF16)
            S.copy(prevb, hst)
            py = psb("B")
            for h in range(4):
                S.mm(py[:, h * 64:(h + 1) * 64], MT[:, h, :], xdt[:, h, :], start=True, stop=False)
                S.mm(py[:, h * 64:(h + 1) * 64], CsT[:, h, :], prevb[:, h, :], start=False, stop=False)
                S.mm(py[:, h * 64:(h + 1) * 64], diagD[:, h, :], xs[:, h * 64:(h + 1) * 64], start=False, stop=True)
            pst = psb("C")
            for h in range(4):
                S.mm(pst[:, h * 64:(h + 1) * 64], Bp[:, h // 2, :], xdd[:, h, :])
            for h in range(4):
                S.stt(hst[:, h, :], hst[:, h, :], cd[:, h:h + 1], pst[:, h * 64:(h + 1) * 64], ALU.mult, ALU.add)
            if stop <= 4:
                continue
            yg = rot("yg", [128, 256], F32)
            S.tt(yg, py[:, 0:256], zs, ALU.mult)
            ss = rot("yss", [128, 1], F32)
            ysq = rot("ysq", [128, 256], F32)
            S.tt(ysq, yg, yg, ALU.mult)
            S.op("dve", lambda e, o=ss.ap, i=ysq.ap: e.reduce_sum(o, i, AX.X), reads=[ysq], writes=[ss])
            rs = rot("yrs", [128, 1], F32)
            rstd_from(rs, ss, 1.0 / 256)
            yo = rot("yo", [128, 256], F32)
            S.stt(yo, yg, rs, gn, ALU.mult, ALU.mult)
            if stop <= 5:
                continue
            pt = psb("A")
            for j in range(2):
                S.transpose(pt[:, j * 128:(j + 1) * 128], yo[:, j * 128:(j + 1) * 128], ident)
            if stop <= 6:
                continue
            for j in range(2):
                S.copy(V(mixT_ap[:, 6 + j, t0:t0 + 128], mixT_t[6 + j][tt // 4]), pt[:, j * 128:(j + 1) * 128],
                       eng="dve")
        arena_reset(ssd_t)

    def out_proj_and_norm2(l, s, src_ap_fn, src_tile_fn, g2):
        nonlocal wbuf_t
        dst, tiles = WB16[("w_out", l)]
        wbuf_t = S.retile(wbuf_t, 1)
        wo = V(WBUF.rearrange("p (k c) -> p k c", k=8), wbuf_t[0])
        S.dma(wo, V(dst.rearrange("(k p) c -> p k c", p=128), CONST), extra_reads=tiles)
        for tt in range(NT):
            xt = load_x_tile(src_ap_fn(tt), src_tile_fn(tt))
            for half in range(2):
                pw = psb("B")
                for k in range(8):
                    S.mm(pw, V(mixT_ap[:, k, tt * 128:(tt + 1) * 128], mixT_t[k][tt // 4]),
                         wo[:, k, half * 512:(half + 1) * 512], start=(k == 0), stop=(k == 7))
                S.tt(xt[:, half * 512:(half + 1) * 512], pw, xt[:, half * 512:(half + 1) * 512], ALU.add)
            S.dma(V(out_d[s, tt * 128:(tt + 1) * 128, :], out_t[s][tt]), xt)
            norm_tile_to_hT(xt, g2, tt)

    def ffn(l, s):
        nonlocal mixT_t
        dwi, twi = WB16[("ffn_w_in", l)]
        dwo, two = WB16[("ffn_w_out", l)]
        dwi_r = dwi.rearrange("(k p) c -> p k c", p=128)
        ar_old = arena_tiles
        newt = S.retile(ar_old, 22 + 4 + 2)
        act_tiles, wo_t, ub_t = newt[0:22], newt[22:26], newt[26:28]
        actT_ap = arena_ap[:, 0:22 * 512].rearrange("p (f t) -> p f t", f=22)
        wo_bufs = [V(arena_ap[:, 11264 + i * 1024: 11264 + (i + 1) * 1024], t) for i, t in enumerate(wo_t)]
        ub_bufs = [V(arena_ap[:, 15360 + i * 516: 15360 + i * 516 + 514], t) for i, t in enumerate(ub_t)]
        mx_old = [t for row in mixT_t for t in row]
        wi_t = S.retile(mx_old, 4)
        mflat = mixT_ap.rearrange("p k t -> p (k t)")
        wi_bufs = [V(mflat[:, i * 4096:(i + 1) * 4096].rearrange("p (k c) -> p k c", k=8), t)
                   for i, t in enumerate(wi_t)]
        cwr = rot("cwr", [44, 3, 128], F32, 1)
        S.dma(cwr, V(W["ffn_conv_w"][l].rearrange("t (c p) -> c t p", p=128), CONST))
        cbr = rot("cbr", [44, 128], F32, 1)
        S.dma(cbr, V(W["ffn_conv_b"][l].rearrange("(c p) -> c p", p=128), CONST))
        cwf = rot("cwf5", [128, 5, 44], F32, 1)
        for t_ in range(4):
            pt = psb("C")
            src = cwr[:, t_, :] if t_ < 3 else cbr
            S.transpose(pt[:, 0:44], src, ident[0:44, 0:44])
            S.copy(cwf[:, t_, :], pt[:, 0:44])
        S.ts(cwf[:, 4, :], cwf[:, 3, :], -1.0, ALU.mult)
        halo = rot("halo", [128, 44, 2], BF16, 1)
        S.memset(halo, 0.0)
        ubi = [0]

        def conv_part(pu, fi, func):
            ub = ub_bufs[ubi[0] % 2]
            ubi[0] += 1
            dgs = []
            for tap in range(3):
                dg = rot("dgf", [128, 128], BF16, 4)
                S.ts(dg, identb, cwf[:, tap, fi:fi + 1], ALU.mult, eng="pool")
                dgs.append(dg)
            S.copy(ub[:, 0:2], halo[:, fi, :], eng="pool")
            S.copy(ub[:, 2:514], pu, eng="act")
            S.copy(halo[:, fi, :], ub[:, 512:514], eng="pool")
            pc = psb("C")
            for tap in range(3):
                S.mm(pc, dgs[tap], ub[:, tap:tap + 512], start=(tap == 0), stop=(tap == 2))
            if func == AF.Silu:
                e = rot("fe", [128, 512], F32, 2)
                S.act(e, pc, AF.Exp, bias=cwf[:, 4, fi:fi + 1], scale=-1.0)
                S.act(e, e, AF.Ln, bias=ONE_AP)
                S.act(e, e, AF.Exp, scale=-1.0)
                res = rot("cres0", [128, 512], F32, 2)
                S.stt(res, pc, cwf[:, 3, fi:fi + 1], e, ALU.add, ALU.mult)
                return res
            return pc

        pend = [None]
        gres = {}

        def finish(pu, part, fc):
            r_ = conv_part(pu, part * 22 + fc, AF.Silu if part == 0 else AF.Identity)
            if part == 0:
                gres[fc] = r_
            else:
                S.stt(V(actT_ap[:, fc, :], act_tiles[fc]), r_, cwf[:, 3, 22 + fc:22 + fc + 1], gres.pop(fc),
                      ALU.add, ALU.mult)

        groups = [(g0, min(4, 22 - g0)) for g0 in range(0, 22, 4)]
        for b in range(NQB):
            for gi, (g0, ng) in enumerate(groups):
                wg = wi_bufs[(gi % 2) * 2]
                wu = wi_bufs[(gi % 2) * 2 + 1]
                S.dma(wg[:, :, 0:ng * 128], V(dwi_r[:, :, g0 * 128:(g0 + ng) * 128], CONST), extra_reads=twi)
                S.dma(wu[:, :, 0:ng * 128], V(dwi_r[:, :, 2816 + g0 * 128:2816 + (g0 + ng) * 128], CONST),
                      extra_reads=twi)
                for j in range(ng):
                    fc = g0 + j
                    for part, wsrc in ((0, wg), (1, wu)):
                        pu = psb("B")
                        for k in range(8):
                            S.mm(pu, wsrc[:, k, j * 128:(j + 1) * 128], hTv(k, b * 512, (b + 1) * 512),
                                 start=(k == 0), stop=(k == 7))
                        if pend[0] is not None:
                            finish(*pend[0])
                        pend[0] = (pu, part, fc)
            finish(*pend[0])
            pend[0] = None
            for fc in range(22):
                wo = wo_bufs[fc % 4]
                S.dma(wo, V(dwo[fc * 128:(fc + 1) * 128, :], CONST), extra_reads=two)
                for j in range(4):
                    for half in range(2):
                        S.mm(PS[j * 2 + half], V(actT_ap[:, fc, j * 128:(j + 1) * 128], act_tiles[fc]),
                             wo[:, half * 512:(half + 1) * 512], start=(fc == 0), stop=(fc == 21))
            for j in range(4):
                tt = 4 * b + j
                xt = load_x_tile(out_d[s, tt * 128:(tt + 1) * 128, :], out_t[s][tt])
                for half in range(2):
                    S.tt(xt[:, half * 512:(half + 1) * 512], PS[j * 2 + half], xt[:, half * 512:(half + 1) * 512],
                         ALU.add)
                S.dma(V(out_d[s, tt * 128:(tt + 1) * 128, :], out_t[s][tt]), xt)
        arena_reset(newt)
        new = S.retile(wi_t, 32)
        mixT_t = [[new[c * 4 + b] for b in range(NQB)] for c in range(8)]

    if dbg:
        for c in range(8):
            for b in range(NQB):
                S.memset(V(mixT_ap[:, c, b * 512:(b + 1) * 512], mixT_t[c][b]), 0.0)
    for s in range(NS):
        rope_tables(s)
    for s in range(NS):
        cur_seq[0] = s
        for l in range(NL):
            P = layer_params(l)
            S.dma(gpp[:, 0:8], V(W["mix_norm_g"][l].rearrange("(k p) -> p k", p=128), CONST),
                  allow_slow_non_contiguous=True)
            S.dma(gpp[:, 8:16], V(W["ffn_norm_g"][l].rearrange("(k p) -> p k", p=128), CONST),
                  allow_slow_non_contiguous=True)
            g1, g2 = 0, 1
            if l == 0:
                src_ap = lambda tt, s=s: x_d[s, tt * 128:(tt + 1) * 128, :]
                src_tile = lambda tt: CONST
            else:
                src_ap = lambda tt, s=s: out_d[s, tt * 128:(tt + 1) * 128, :]
                src_tile = lambda tt, s=s: out_t[s][tt]
            for tt in range(NT):
                xt = load_x_tile(src_ap(tt), src_tile(tt))
                norm_tile_to_hT(xt, g1, tt)
            if dbg and s == 0 and l == 0:
                allh = S.retile(hT_t, 1)[0]
                dbgdump("hT", V(hT_ap, allh), [128, 8, SEQ])
                hT_t[:] = S.retile([allh], NQB)
            if "moba" in phases:
                moba(l, s, P)
            if "mla" in phases:
                mla(l, s, P)
            if "sb" in phases:
                sbmix(l, s, P)
            if "ssd" in phases:
                ssd(l, s, P)
            if dbg and s == 0 and l == 0:
                allmix = S.retile([t for row in mixT_t for t in row], 1)[0]
                dbgdump("mixT", V(mixT_ap, allmix), [128, 8, SEQ])
                new = S.retile([allmix], 32)
                mixT_t = [[new[c * 4 + b] for b in range(NQB)] for c in range(8)]
            if "ffn" in phases:
                out_proj_and_norm2(l, s, src_ap, src_tile, g2)
                ffn(l, s)
    S.emit()
    return nc, dbg_out


_CACHE = {}


def kernel(**inputs):
    n = 8
    NS = 2
    if "nc" not in _CACHE:
        _CACHE["nc"] = build(2, NS)[0]
    nc = _CACHE["nc"]
    hc = host_consts()
    x = np.ascontiguousarray(inputs["x"], dtype=np.float32)
    pos = np.ascontiguousarray(inputs["positions"], dtype=np.int32)
    in_maps = []
    for c in range(n):
        m = {"x": x[c * NS:(c + 1) * NS], "positions": pos[c * NS:(c + 1) * NS]}
        for w in WNAMES:
            m[w] = np.ascontiguousarray(inputs[w], dtype=np.float32)
        for k, a in hc.items():
            m["c_" + k] = a
        in_maps.append(m)
    res = run_bass_kernel_spmd(nc, in_maps, core_ids=list(range(n)))
    out = np.concatenate([np.asarray(r["out"], dtype=np.float32) for r in res.results], axis=0)
    return out
```
